# Optimizing a Trainium2 kernel written in Bass

```python
import math
import jax, jax.numpy as jnp
from jax import lax
import numpy as np

D_MODEL = 1024
BATCH = 32
SEQ = 2048
DEPTH = 1

HEAD_DIM = 128
ATTN_SLOTS = 4
DIL_PATTERNS = ((128, 1), (512, 4), (2048, 16))
N_DIL = len(DIL_PATTERNS)
ATTN_QK = N_DIL * ATTN_SLOTS * HEAD_DIM
ATTN_OUT = ATTN_SLOTS * HEAD_DIM
BAND_BLOCK = 128
ROPE_THETA = 10000.0
CONV_CH = 512
CONV_WIDTH = 31
MEM_LEN = 256
MEM_HEADS = 4
MEM_HEAD_DIM = 128
MEM_W = MEM_HEADS * MEM_HEAD_DIM
N_BRANCH = 3
IN_COLS = 3 * ATTN_QK + 2 * CONV_CH + MEM_W + N_BRANCH * D_MODEL
N_GROUPS = 4
EXPERTS_PER_GROUP = 4
N_EXPERTS = N_GROUPS * EXPERTS_PER_GROUP
TOP_K = 2
EXPERT_FF = 512
LN_EPS = 1e-5
DN_ALPHA = (2.0 * DEPTH) ** 0.25
DN_BETA = (8.0 * DEPTH) ** -0.25

kernel_name = "hybrid_dilated_conformer_memory_hmoe_deepnorm"


def layer_norm(x, g, b):
    xf = x.astype(jnp.float32)
    mu = jnp.mean(xf, axis=-1, keepdims=True)
    var = jnp.mean(jnp.square(xf - mu), axis=-1, keepdims=True)
    y = (xf - mu) * lax.rsqrt(var + LN_EPS)
    return (y * g.astype(jnp.float32) + b.astype(jnp.float32)).astype(x.dtype)


def rope_tables(positions):
    half = HEAD_DIM // 2
    inv = ROPE_THETA ** (-jnp.arange(half, dtype=jnp.float32) / half)
    ang = positions.astype(jnp.float32)[..., None] * inv
    return jnp.cos(ang)[:, :, None, :], jnp.sin(ang)[:, :, None, :]


def apply_rope(t, cos, sin):
    half = HEAD_DIM // 2
    tf = t.astype(jnp.float32)
    t1, t2 = tf[..., :half], tf[..., half:]
    return jnp.concatenate([t1 * cos - t2 * sin, t2 * cos + t1 * sin], axis=-1).astype(t.dtype)


def dilated_group_attention(q, k, v, window, dilation):
    B, S, H, E = q.shape
    L = S // dilation
    nb = -(-L // BAND_BLOCK)
    Lp = nb * BAND_BLOCK
    reach = window // dilation

    def split(t):
        t = t.reshape(B, L, dilation, H, E).transpose(0, 2, 3, 1, 4)
        t = jnp.pad(t, ((0, 0), (0, 0), (0, 0), (0, Lp - L), (0, 0)))
        return t.reshape(B, dilation, H, nb, BAND_BLOCK, E)

    def with_prev(t):
        prev = jnp.pad(t, ((0, 0), (0, 0), (0, 0), (1, 0), (0, 0), (0, 0)))[:, :, :, :-1]
        return jnp.concatenate([prev, t], axis=4)

    qb = split(q)
    kk = with_prev(split(k))
    vv = with_prev(split(v))
    s = jnp.einsum('brhnqe,brhnke->brhnqk', qb, kk).astype(jnp.float32) * (E ** -0.5)
    qi = jnp.arange(BAND_BLOCK)[:, None]
    kj = jnp.arange(2 * BAND_BLOCK)[None, :]
    dist = BAND_BLOCK + qi - kj
    blk = jnp.arange(nb)[:, None, None]
    valid = (dist >= 0) & (dist <= reach) & (blk * BAND_BLOCK + kj - BAND_BLOCK >= 0)
    s = jnp.where(valid, s, -jnp.inf)
    m = jnp.max(s, axis=-1, keepdims=True)
    lse = m + jnp.log(jnp.sum(jnp.exp(s - m), axis=-1, keepdims=True))
    p = jnp.exp(s - lse)
    o = jnp.einsum('brhnqk,brhnke->brhnqe', p.astype(v.dtype), vv)
    o = o.reshape(B, dilation, H, Lp, E)[:, :, :, :L].transpose(0, 3, 1, 2, 4).reshape(B, S, H, E)
    lse = lse.reshape(B, dilation, H, Lp)[:, :, :, :L].transpose(0, 3, 1, 2).reshape(B, S, H)
    return o, lse


def hybrid_mixer(h, mem, cos, sin, w_in, b_in, conv_w, conv_b, conv_ln_g, conv_ln_b,
                 w_mem_kv, w_attn_o, w_conv_o, w_mem_o, w_out):
    B, S, D = h.shape
    u = h @ w_in + b_in
    cuts = [ATTN_QK, 2 * ATTN_QK, 3 * ATTN_QK, 3 * ATTN_QK + 2 * CONV_CH,
            3 * ATTN_QK + 2 * CONV_CH + MEM_W]
    q, k, v, glu_in, qm, gate_pre = jnp.split(u, cuts, axis=-1)

    nh = N_DIL * ATTN_SLOTS
    q = apply_rope(q.reshape(B, S, nh, HEAD_DIM), cos, sin).reshape(B, S, N_DIL, ATTN_SLOTS, HEAD_DIM)
    k = apply_rope(k.reshape(B, S, nh, HEAD_DIM), cos, sin).reshape(B, S, N_DIL, ATTN_SLOTS, HEAD_DIM)
    v = v.reshape(B, S, N_DIL, ATTN_SLOTS, HEAD_DIM)
    outs, lses = [], []
    for g, (window, dilation) in enumerate(DIL_PATTERNS):
        o_g, lse_g = dilated_group_attention(q[:, :, g], k[:, :, g], v[:, :, g], window, dilation)
        outs.append(o_g)
        lses.append(lse_g)
    wts = jax.nn.softmax(jnp.stack(lses, axis=0), axis=0)
    o_attn = jnp.sum(wts[..., None].astype(v.dtype) * jnp.stack(outs, axis=0), axis=0)
    y_attn = o_attn.reshape(B, S, ATTN_OUT) @ w_attn_o

    a, b = jnp.split(glu_in, 2, axis=-1)
    c = a * jax.nn.sigmoid(b)
    c = lax.conv_general_dilated(c, conv_w[:, None, :], window_strides=(1,),
                                 padding=[(CONV_WIDTH - 1, 0)],
                                 dimension_numbers=('NWC', 'WIO', 'NWC'),
                                 feature_group_count=CONV_CH) + conv_b
    c = jax.nn.silu(layer_norm(c, conv_ln_g, conv_ln_b))
    y_conv = c @ w_conv_o

    kv = mem @ w_mem_kv
    km, vm = jnp.split(kv, 2, axis=-1)
    Mlen = mem.shape[1]
    qm = qm.reshape(B, S, MEM_HEADS, MEM_HEAD_DIM)
    km = km.reshape(B, Mlen, MEM_HEADS, MEM_HEAD_DIM)
    vm = vm.reshape(B, Mlen, MEM_HEADS, MEM_HEAD_DIM)
    sm = jnp.einsum('bshe,bmhe->bhsm', qm, km).astype(jnp.float32) * (MEM_HEAD_DIM ** -0.5)
    pm = jax.nn.softmax(sm, axis=-1)
    om = jnp.einsum('bhsm,bmhe->bshe', pm.astype(vm.dtype), vm).reshape(B, S, MEM_W)
    y_mem = om @ w_mem_o

    gates = jax.nn.sigmoid(gate_pre).reshape(B, S, N_BRANCH, D)
    merged = gates[:, :, 0] * y_attn + gates[:, :, 1] * y_conv + gates[:, :, 2] * y_mem
    return merged @ w_out


def hierarchical_moe(h, w_gr, b_gr, w_er, b_er, w_g, w_u, w_d):
    B, S, D = h.shape
    t = h.reshape(B * S, D)
    gl = (t @ w_gr).astype(jnp.float32) + b_gr.astype(jnp.float32)
    gp = jax.nn.softmax(gl, axis=-1)
    g_sel = jnp.argmax(gl, axis=-1)
    g_prob = jnp.take_along_axis(gp, g_sel[:, None], axis=-1)[:, 0]
    el = jnp.einsum('td,gde->tge', t, w_er).astype(jnp.float32) + b_er.astype(jnp.float32)
    el_sel = jnp.take_along_axis(el, g_sel[:, None, None], axis=1)[:, 0]
    top_v, top_i = lax.top_k(el_sel, TOP_K)
    top_w = jax.nn.softmax(top_v, axis=-1) * g_prob[:, None]
    ew = jnp.sum(jax.nn.one_hot(top_i, EXPERTS_PER_GROUP, dtype=jnp.float32) * top_w[..., None], axis=1)
    full = (jax.nn.one_hot(g_sel, N_GROUPS, dtype=jnp.float32)[:, :, None] * ew[:, None, :])
    full = full.reshape(B * S, N_EXPERTS).astype(t.dtype)
    y = jnp.zeros_like(t)
    for e in range(N_EXPERTS):
        hid = jax.nn.silu(t @ w_g[e]) * (t @ w_u[e])
        y = y + full[:, e:e + 1] * (hid @ w_d[e])
    return y.reshape(B, S, D)


def setup_inputs(seed: int = 0) -> dict:
    key = jax.random.key(seed)
    ks = jax.random.split(key, 26)
    f32 = jnp.float32
    nrm = lambda k, shape, scale: jax.random.normal(k, shape, f32) * scale
    x = jax.random.normal(ks[0], (BATCH, SEQ, D_MODEL), f32)
    mem = jax.random.normal(ks[1], (BATCH, MEM_LEN, D_MODEL), f32)
    offset = jax.random.randint(ks[2], (BATCH, 1), 0, 1024, dtype=jnp.int32)
    positions = offset + jnp.arange(SEQ, dtype=jnp.int32)[None, :]
    w_in = nrm(ks[3], (DEPTH, D_MODEL, IN_COLS), D_MODEL ** -0.5)
    w_in = w_in.at[:, :, 2 * ATTN_QK:3 * ATTN_QK].multiply(DN_BETA)
    b_in = nrm(ks[4], (DEPTH, IN_COLS), 0.02)
    conv_w = nrm(ks[5], (DEPTH, CONV_WIDTH, CONV_CH), CONV_WIDTH ** -0.5)
    conv_b = nrm(ks[6], (DEPTH, CONV_CH), 0.02)
    conv_ln_g = 1.0 + nrm(ks[7], (DEPTH, CONV_CH), 0.02)
    conv_ln_b = nrm(ks[8], (DEPTH, CONV_CH), 0.02)
    w_mem_kv = nrm(ks[9], (DEPTH, D_MODEL, 2 * MEM_W), D_MODEL ** -0.5)
    w_attn_o = nrm(ks[10], (DEPTH, ATTN_OUT, D_MODEL), ATTN_OUT ** -0.5 * DN_BETA)
    w_conv_o = nrm(ks[11], (DEPTH, CONV_CH, D_MODEL), CONV_CH ** -0.5 * DN_BETA)
    w_mem_o = nrm(ks[12], (DEPTH, MEM_W, D_MODEL), MEM_W ** -0.5 * DN_BETA)
    w_out = nrm(ks[13], (DEPTH, D_MODEL, D_MODEL), D_MODEL ** -0.5 * DN_BETA)
    ln1_g = 1.0 + nrm(ks[14], (DEPTH, D_MODEL), 0.02)
    ln1_b = nrm(ks[15], (DEPTH, D_MODEL), 0.02)
    w_group_router = nrm(ks[16], (DEPTH, D_MODEL, N_GROUPS), D_MODEL ** -0.5)
    b_group_router = nrm(ks[17], (DEPTH, N_GROUPS), 0.01)
    w_expert_router = nrm(ks[18], (DEPTH, N_GROUPS, D_MODEL, EXPERTS_PER_GROUP), D_MODEL ** -0.5)
    b_expert_router = nrm(ks[19], (DEPTH, N_GROUPS, EXPERTS_PER_GROUP), 0.01)
    w_exp_gate = nrm(ks[20], (DEPTH, N_EXPERTS, D_MODEL, EXPERT_FF), D_MODEL ** -0.5)
    w_exp_up = nrm(ks[21], (DEPTH, N_EXPERTS, D_MODEL, EXPERT_FF), D_MODEL ** -0.5)
    w_exp_down = nrm(ks[22], (DEPTH, N_EXPERTS, EXPERT_FF, D_MODEL), EXPERT_FF ** -0.5 * DN_BETA)
    ln2_g = 1.0 + nrm(ks[23], (DEPTH, D_MODEL), 0.02)
    ln2_b = nrm(ks[24], (DEPTH, D_MODEL), 0.02)
    return {"x": x, "mem": mem, "positions": positions, "w_in": w_in, "b_in": b_in,
            "conv_w": conv_w, "conv_b": conv_b, "conv_ln_g": conv_ln_g, "conv_ln_b": conv_ln_b,
            "w_mem_kv": w_mem_kv, "w_attn_o": w_attn_o, "w_conv_o": w_conv_o, "w_mem_o": w_mem_o,
            "w_out": w_out, "ln1_g": ln1_g, "ln1_b": ln1_b,
            "w_group_router": w_group_router, "b_group_router": b_group_router,
            "w_expert_router": w_expert_router, "b_expert_router": b_expert_router,
            "w_exp_gate": w_exp_gate, "w_exp_up": w_exp_up, "w_exp_down": w_exp_down,
            "ln2_g": ln2_g, "ln2_b": ln2_b}


def reference(x, mem, positions, w_in, b_in, conv_w, conv_b, conv_ln_g, conv_ln_b,
              w_mem_kv, w_attn_o, w_conv_o, w_mem_o, w_out, ln1_g, ln1_b,
              w_group_router, b_group_router, w_expert_router, b_expert_router,
              w_exp_gate, w_exp_up, w_exp_down, ln2_g, ln2_b):
    cos, sin = rope_tables(positions)
    for l in range(DEPTH):
        mix = hybrid_mixer(x, mem, cos, sin, w_in[l], b_in[l], conv_w[l], conv_b[l],
                           conv_ln_g[l], conv_ln_b[l], w_mem_kv[l], w_attn_o[l], w_conv_o[l],
                           w_mem_o[l], w_out[l])
        x = layer_norm(DN_ALPHA * x + mix, ln1_g[l], ln1_b[l])
        ffn = hierarchical_moe(x, w_group_router[l], b_group_router[l], w_expert_router[l],
                               b_expert_router[l], w_exp_gate[l], w_exp_up[l], w_exp_down[l])
        x = layer_norm(DN_ALPHA * x + ffn, ln2_g[l], ln2_b[l])
    return x
```

```python
import math
from contextlib import ExitStack

import numpy as np
import ml_dtypes

import concourse.bass as bass
import concourse.mybir as mybir
from concourse.bass_utils import run_bass_kernel_spmd

F32 = mybir.dt.float32
BF16 = mybir.dt.bfloat16
I32 = mybir.dt.int32
ALU = mybir.AluOpType
AF = mybir.ActivationFunctionType
AX = mybir.AxisListType

NCORES = 8
S = 2048
D = 1024
MEM = 256
NT = S // 128
NTB = S // 512
DIL = (1, 4, 16)
ALPHA = 2.0 ** 0.25
EPS = 1e-5
SCALE = 128.0 ** -0.5
NEG = -30000.0
TWO_PI = 2.0 * math.pi
C1 = 6.28125
C2 = TWO_PI - C1
ARENA_BYTES = 204 * 1024
NSLOT = 48
SPARSE = True
SKIP_UNUSED = False


def _dsize(dt):
    return 2 if dt == BF16 else 4


class Res:
    __slots__ = ("name", "w", "r", "dsem", "excl")

    def __init__(self, name, excl=False):
        self.name = name
        self.excl = excl
        self.w = None
        self.r = {}
        self.dsem = None


class T:
    __slots__ = ("ap", "res")

    def __init__(self, ap, res):
        self.ap = ap
        self.res = res


class Eng:
    def __init__(self, name, h, sem):
        self.name = name
        self.h = h
        self.sem = sem
        self.cnt = 0
        self.pending = False
        self.waited = {}


class KB:
    def __init__(self, nc, es):
        self.nc = nc
        self.es = es
        self.E = {}
        for n, h in (("pe", nc.tensor), ("act", nc.scalar), ("dve", nc.vector),
                     ("pool", nc.gpsimd), ("sp", nc.sync)):
            sem = es.enter_context(nc.semaphore("sem_" + n))
            self.E[n] = Eng(n, h, sem)
        self.semown = {id(e.sem): e for e in self.E.values()}
        self.dsems = []
        self.dsem_by_name = {}
        self.nsem = 0

    def _wait(self, eng, ev):
        sem, val = ev
        k = id(sem)
        if eng.waited.get(k, 0) >= val:
            return
        own = self.semown.get(k)
        if own is not None:
            assert val <= own.cnt, "wait on a signal that was never issued (%s)" % own.name
        eng.h.wait_ge(sem, val)
        eng.waited[k] = val

    def _deps(self, reads, writes):
        evs = []
        for r in reads:
            if r.w is not None:
                evs.append(r.w)
        for w in writes:
            if w.w is not None:
                evs.append(w.w)
            evs.extend(w.r.values())
        return evs

    def op(self, en, fn, reads=(), writes=(), signal=True):
        eng = self.E[en]
        reads = [t.res if isinstance(t, T) else t for t in reads]
        writes = [t.res if isinstance(t, T) else t for t in writes]
        writes = writes + [r for r in reads if r.excl and r not in writes]
        for ev in self._deps(reads, writes):
            if ev[0] is eng.sem and en == "pe":
                continue
            self._wait(eng, ev)
        ins = fn(eng.h)
        if signal:
            eng.cnt += 1
            ins.then_inc(eng.sem, 1)
            ev = (eng.sem, eng.cnt)
            eng.pending = False
        else:
            ev = (eng.sem, eng.cnt + 1)
            eng.pending = True
        k = id(eng.sem)
        for r in reads:
            r.r[k] = ev
        for w in writes:
            w.w = ev
            w.r = {}

    def _dsem(self, sres, en):
        key = (sres.name, en)
        ds = self.dsem_by_name.get(key)
        if ds is None:
            self.nsem += 1
            ds = [self.es.enter_context(self.nc.semaphore("dsem%d" % self.nsem)), 0]
            self.dsems.append(ds)
            self.dsem_by_name[key] = ds
        return ds

    def dma(self, en, pairs, sres, reads=(), writes=()):
        eng = self.E[en]
        sres = sres.res if isinstance(sres, T) else sres
        reads = [t.res if isinstance(t, T) else t for t in reads]
        writes = [t.res if isinstance(t, T) else t for t in writes]
        for ev in self._deps(reads, writes):
            self._wait(eng, ev)
        ds = self._dsem(sres, en)
        for o, i in pairs:
            eng.h.dma_start(out=o, in_=i).then_inc(ds[0], 16)
            ds[1] += 16
        ev = (ds[0], ds[1])
        k = id(ev[0])
        for r in reads:
            r.r[k] = ev
        for w in writes:
            w.w = ev
            w.r = {}

    def idma(self, out, out_idx, in_, in_idx, sres, bound, reads=(), writes=()):
        eng = self.E["pool"]
        sres = sres.res if isinstance(sres, T) else sres
        reads = [t.res if isinstance(t, T) else t for t in reads]
        writes = [t.res if isinstance(t, T) else t for t in writes]
        for ev in self._deps(reads, writes):
            self._wait(eng, ev)
        ds = self._dsem(sres, "pool_ind")
        oo = bass.IndirectOffsetOnAxis(ap=out_idx, axis=0) if out_idx is not None else None
        io = bass.IndirectOffsetOnAxis(ap=in_idx, axis=0) if in_idx is not None else None
        if bound is None:
            eng.h.indirect_dma_start(out=out, out_offset=oo, in_=in_, in_offset=io).then_inc(ds[0], 16)
        else:
            eng.h.indirect_dma_start(out=out, out_offset=oo, in_=in_, in_offset=io, bounds_check=bound,
                                     oob_is_err=False).then_inc(ds[0], 16)
        ds[1] += 16
        ev = (ds[0], ds[1])
        k = id(ev[0])
        for r in reads:
            r.r[k] = ev
        for w in writes:
            w.w = ev
            w.r = {}

    def barrier(self, engines=None):
        for e in self.E.values():
            assert not e.pending
        for e in self.E.values():
            for x in self.E.values():
                if x is not e and x.cnt > 0:
                    self._wait(e, (x.sem, x.cnt))
            for ds in self.dsems:
                if ds[1] > 0:
                    self._wait(e, (ds[0], ds[1]))

    def final_wait(self):
        e = self.E["sp"]
        for ds in self.dsems:
            if ds[1] > 0:
                self._wait(e, (ds[0], ds[1]))


class Arena:
    def __init__(self, ap):
        self.ap = ap
        self.off = 0
        self.peak = 0
        self.top = ARENA_BYTES
        self.where = {}

    def alloc(self, name, shape, dt, top=False):
        n = 1
        for s in shape[1:]:
            n *= s
        nb = n * _dsize(dt)
        nb = (nb + 63) // 64 * 64
        if top:
            self.top -= nb
            start = self.top
        else:
            start = self.off
            self.off += nb
        assert self.off <= self.top, "arena overflow at %s: %d/%d" % (name, self.off, self.top)
        v = self.ap[:, start // 4:(start + nb) // 4]
        if dt != F32:
            v = v.bitcast(dt)
        v = v[:, 0:n]
        if len(shape) == 3:
            v = v.rearrange("p (a b) -> p a b", a=shape[1])
        elif len(shape) == 4:
            v = v.rearrange("p (a b c) -> p a b c", a=shape[1], b=shape[2])
        if shape[0] != 128:
            v = v[0:shape[0]]
        self.peak = max(self.peak, self.off + ARENA_BYTES - self.top)
        self.where[name] = (start, n, dt, tuple(shape))
        return T(v, Res(name))

    def mark(self):
        return self.off

    def release(self, m):
        self.off = m


class _Stop(Exception):
    pass


def build_program(nb, stage=None):
    nc = bass.Bass("TRN2", target_bir_lowering=False)

    def din(name, shape, dt=F32):
        return nc.dram_tensor(name, list(shape), dt, kind="ExternalInput").ap()

    x_d = din("x", [nb, S, D])
    mem_d = din("mem", [nb, MEM, D])
    pos_d = din("pos", [nb, S], I32)
    win_d = din("w_in_r", [72, 128, 8, 128])
    binT_d = din("b_inT", [128, 72])
    bv_d = din("b_v", [1, 1536])
    convw_d = din("conv_wT", [128, 4, 31])
    convp_d = din("conv_p", [128, 3, 4])
    wkv_d = din("w_kv_r", [128, 8, 1024])
    wao_d = din("w_ao_r", [128, 4, 1024])
    wco_d = din("w_co_r", [128, 4, 1024])
    wmo_d = din("w_mo_r", [128, 4, 1024])
    wout_d = din("w_out_r", [128, 8, 1024])
    lnp_d = din("ln_p", [4, 1024])
    wr_d = din("w_r_r", [128, 8, 20])
    br_d = din("b_r", [1, 20])
    wg_d = din("w_g_r", [16, 128, 8, 512])
    wu_d = din("w_u_r", [16, 128, 8, 512])
    wd_d = din("w_d_r", [16, 128, 4, 1024])
    cbf_d = din("c_bf", [128, 9, 128], BF16)
    iota_d = din("c_iota", [128, NSLOT])
    cf_d = din("c_f32", [128, 4])
    onesf_d = din("c_onesf", [128, 128])
    out_d = nc.dram_tensor("out", [nb, S, D], F32, kind="ExternalOutput").ap()
    x1s_d = nc.dram_tensor("x1_scratch", [nb, S, D], F32, kind="Internal").ap()
    x1s_res = [[Res("x1s%d_%d" % (b, t)) for t in range(NT)] for b in range(nb)]
    x1h_d = nc.dram_tensor("x1h_scratch", [nb, S, D], BF16, kind="Internal").ap()
    x1h_res = [[Res("x1h%d_%d" % (b, t)) for t in range(NT)] for b in range(nb)]
    xs_d = nc.dram_tensor("xs_scratch", [NSLOT * 128, D], BF16, kind="Internal").ap()
    ys_d = nc.dram_tensor("ys_scratch", [NSLOT * 128, D], F32, kind="Internal").ap()
    wgb_d = nc.dram_tensor("wgb_scratch", [16 * 128, 4096], BF16, kind="Internal").ap()
    wub_d = nc.dram_tensor("wub_scratch", [16 * 128, 4096], BF16, kind="Internal").ap()
    wdb_d = nc.dram_tensor("wdb_scratch", [16 * 128, 4096], BF16, kind="Internal").ap()

    with ExitStack() as es:
        K = KB(nc, es)
        arena_t = es.enter_context(nc.sbuf_tensor("arena", [128, ARENA_BYTES // 4], F32))
        A = Arena(arena_t[:])
        PS = []
        for i in range(8):
            pt = es.enter_context(nc.psum_tensor("ps%d" % i, [128, 512], F32))
            PS.append(T(pt[:], Res("ps%d" % i, excl=True)))

        def psb(i):
            return PS[i].ap.bitcast(BF16)

        cbf = A.alloc("cbf", [128, 9, 128], BF16)
        iota = A.alloc("iota", [128, NSLOT], F32)
        cf = A.alloc("cf", [128, 4], F32)
        onesf = A.alloc("onesf", [128, 128], F32)
        binT = A.alloc("binT", [128, 72], F32)
        convw = A.alloc("convw", [128, 4, 31], F32)
        convp = A.alloc("convp", [128, 3, 4], F32)
        lnp = A.alloc("lnp", [128, 4, 1024], F32)
        wr_hi = A.alloc("wr_hi", [128, 8, 20], BF16)
        wr_lo = A.alloc("wr_lo", [128, 8, 20], BF16)
        wr_f = A.alloc("wr_f", [128, 8, 20], F32)
        wr_t = A.alloc("wr_t", [128, 8, 20], F32)
        brb = A.alloc("brb", [128, 20], F32)
        K.dma("sp", [(cbf.ap, cbf_d)], cbf, writes=[cbf])
        K.dma("sp", [(cf.ap, cf_d)], cf, writes=[cf])
        K.dma("sp", [(iota.ap, iota_d)], iota, writes=[iota])
        K.dma("sp", [(onesf.ap, onesf_d)], onesf, writes=[onesf])
        K.dma("sp", [(binT.ap, binT_d)], binT, writes=[binT])
        K.dma("sp", [(convw.ap, convw_d)], convw, writes=[convw])
        K.dma("sp", [(convp.ap, convp_d)], convp, writes=[convp])
        K.dma("sp", [(lnp.ap[:, i, :], lnp_d[i:i + 1, :].broadcast_to([128, 1024])) for i in range(4)],
              lnp, writes=[lnp])
        K.dma("sp", [(wr_f.ap, wr_d)], wr_f, writes=[wr_f])
        K.dma("sp", [(brb.ap, br_d.broadcast_to([128, 20]))], brb, writes=[brb])
        K.op("dve", lambda e: e.tensor_copy(out=wr_hi.ap, in_=wr_f.ap), [wr_f], [wr_hi])
        K.op("dve", lambda e: e.tensor_tensor(out=wr_t.ap, in0=wr_f.ap, in1=wr_hi.ap, op=ALU.subtract),
             [wr_f, wr_hi], [wr_t])
        K.op("dve", lambda e: e.tensor_copy(out=wr_lo.ap, in_=wr_t.ap), [wr_t], [wr_lo])
        ident = cbf.ap[:, 0, :]
        perm = cbf.ap[:, 1, :]
        onesb = cbf.ap[:, 2, :]
        m_cur = cbf.ap[:, 3, :]
        m_prev = cbf.ap[:, 4, :]
        m_all = cbf.ap[:, 5, :]
        invf = cf.ap[:, 0:1]
        sgn = cf.ap[:, 1:2]
        pidx = cf.ap[:, 2:3]
        ustrict = cbf.ap[:, 6, :]
        m_cp = cbf.ap[:, 3:5, :].rearrange("p a b -> p (a b)")
        m_ca = cbf.ap[:, 7:9, :].rearrange("p a b -> p (a b)")

        base_mark = A.mark()
        wbound = None
        if SKIP_UNUSED:
            bc_reg = nc.gpsimd.alloc_register("wbound")
            nc.gpsimd.reg_mov(bc_reg, 16 * 128 - 1)
            wbound = nc.gpsimd.snap(bc_reg)
        precast_q = []
        wres = Res("wscratch")
        if SPARSE:
            for ex in range(16):
                for src_, dst_ in ((wg_d, wgb_d), (wu_d, wub_d), (wd_d, wdb_d)):
                    precast_q.append((dst_[ex * 128:(ex + 1) * 128, :], src_[ex].rearrange("p a b -> p (a b)")))
            zt = A.alloc("zt", [128, D], BF16)
            K.op("dve", lambda e: e.memset(zt.ap, 0.0), [], [zt])
            K.dma("sp", [(xs_d[j_ * 128:(j_ + 1) * 128, :], zt.ap) for j_ in range(NSLOT)], zt, reads=[zt])
            base_mark = A.mark()

        def issue_precast(n):
            for _ in range(n):
                if precast_q:
                    o_, i_ = precast_q.pop(0)
                    K.dma("pool", [(o_, i_)], wres)

        def mm(out, lhsT, rhs, start, stop, reads, bank, signal, skip=False):
            if skip:
                K.op("pe", lambda e: e.matmul(out, lhsT, rhs, start=start, stop=stop, skip_group_check=True),
                     reads, [bank], signal=signal)
            else:
                K.op("pe", lambda e: e.matmul(out, lhsT, rhs, start=start, stop=stop),
                     reads, [bank], signal=signal)

        def transpose_to(bank_i, col0, in_ap, reads, signal):
            o = psb(bank_i)[:, col0:col0 + 128]
            K.op("pe", lambda e: e.transpose(o, in_ap, ident), list(reads) + [cbf], [PS[bank_i]],
                 signal=signal)

        evac_flip = [0]

        def copy_any(out, in_, reads, writes):
            evac_flip[0] ^= 1
            if evac_flip[0]:
                K.op("act", lambda e: e.copy(out=out, in_=in_), reads, writes)
            else:
                K.op("dve", lambda e: e.tensor_copy(out=out, in_=in_), reads, writes)

        def ln_stats(v, st):
            stats = st.ap[:, 0:12]
            mv = st.ap[:, 12:14]
            K.op("dve", lambda e: e.bn_stats(out=stats[:, 0:6], in_=v.ap[:, 0:512]), [v], [st])
            K.op("dve", lambda e: e.bn_stats(out=stats[:, 6:12], in_=v.ap[:, 512:1024]), [v], [st])
            K.op("dve", lambda e: e.bn_aggr(out=mv, in_=stats), [st], [st])

        def ln_rstd(st):
            mv = st.ap[:, 12:14]
            K.op("act", lambda e: e.activation(out=st.ap[:, 14:15], in_=mv[:, 1:2], func=AF.Sqrt,
                                               bias=EPS, scale=1.0), [st], [st])
            K.op("dve", lambda e: e.reciprocal(out=st.ap[:, 14:15], in_=st.ap[:, 14:15]), [st], [st])
            K.op("dve", lambda e: e.scalar_tensor_tensor(out=st.ap[:, 15:16], in0=mv[:, 0:1], scalar=-1.0,
                                                         in1=st.ap[:, 14:15], op0=ALU.mult, op1=ALU.mult),
                 [st], [st])

        def ln_norm(v, gi, out_t, tmp, st):
            K.op("act", lambda e: e.activation(out=tmp.ap, in_=v.ap, func=AF.Identity,
                                               bias=st.ap[:, 15:16], scale=st.ap[:, 14:15]), [v, st], [tmp])
            K.op("dve", lambda e: e.tensor_tensor(out=tmp.ap, in0=tmp.ap, in1=lnp.ap[:, gi, :], op=ALU.mult),
                 [tmp, lnp], [tmp])
            K.op("pool", lambda e: e.tensor_tensor(out=out_t.ap, in0=tmp.ap, in1=lnp.ap[:, gi + 1, :],
                                                   op=ALU.add), [tmp, lnp], [out_t])

        def sparse_moe(b, oha, ohb, wts, stage=stage):
            NE = 16
            Mb = A.alloc("Mb", [128, NT * NE], BF16)
            tmpf = A.alloc("tmpf", [128, NT, NE], F32)
            tot = A.alloc("tot", [128, NT, NE], F32)
            cum = A.alloc("cum", [128, NT + 1, NE], F32)
            posf = A.alloc("posf", [128, NT, NE], F32)
            sml = A.alloc("sml", [128, 8, NE], F32)
            smi = A.alloc("smi", [128, NE], I32)
            posa = A.alloc("posa", [128, 2, NT], F32)
            posi = A.alloc("posi", [128, 2, NT], I32)
            cmp3 = A.alloc("cmp3", [128, NSLOT, NE], F32)
            ejf = A.alloc("ejf", [128, NSLOT], F32)
            widx = A.alloc("widx", [128, NSLOT], I32)
            top_save = A.top
            xall = A.alloc("xall", [128, NT, D], BF16, top=True)
            K.dma("sp", [(xall.ap[:, tt, :], x1h_d[b, tt * 128:(tt + 1) * 128, :]) for tt in range(NT)], xall,
                  reads=[x1h_res[b][tt] for tt in range(NT)], writes=[xall])
            dvo = lambda fn_, r_, w_: K.op("dve", fn_, r_, w_)
            dvo(lambda e: e.tensor_tensor(out=tmpf.ap, in0=oha.ap, in1=ohb.ap, op=ALU.add), [oha, ohb], [tmpf])
            dvo(lambda e: e.tensor_copy(out=Mb.ap, in_=tmpf.ap.rearrange("p t e -> p (t e)")), [tmpf], [Mb])
            mm(PS[0].ap[:, 0:NT * NE], ustrict, Mb.ap, True, True, [cbf, Mb], PS[0], True)
            mm(PS[1].ap[:, 0:NT * NE], onesb, Mb.ap, True, True, [cbf, Mb], PS[1], True)
            dvo(lambda e: e.tensor_copy(out=tot.ap.rearrange("p t e -> p (t e)"), in_=PS[1].ap[:, 0:NT * NE]),
                [PS[1]], [tot])
            dvo(lambda e: e.memset(cum.ap[:, 0, :], 0.0), [], [cum])
            for tt in range(NT):
                dvo(lambda e: e.tensor_tensor(out=cum.ap[:, tt + 1, :], in0=cum.ap[:, tt, :], in1=tot.ap[:, tt, :],
                                              op=ALU.add), [cum, tot], [cum])
            cnt_ = cum.ap[:, NT, :]
            ntf, endt, base_ = sml.ap[:, 0, :], sml.ap[:, 1, :], sml.ap[:, 2, :]
            dvo(lambda e: e.tensor_scalar(out=sml.ap[:, 3, :], in0=cnt_, scalar1=1.0 / 128.0,
                                          scalar2=127.0 / 128.0 - 0.5 + 1.0 / 256.0, op0=ALU.mult, op1=ALU.add),
                [cum], [sml])
            dvo(lambda e: e.tensor_copy(out=smi.ap, in_=sml.ap[:, 3, :]), [sml], [smi])
            dvo(lambda e: e.tensor_copy(out=ntf, in_=smi.ap), [smi], [sml])
            dvo(lambda e: e.tensor_copy(out=endt[:, 0:1], in_=ntf[:, 0:1]), [sml], [sml])
            for ex in range(1, NE):
                dvo(lambda e: e.tensor_tensor(out=endt[:, ex:ex + 1], in0=endt[:, ex - 1:ex], in1=ntf[:, ex:ex + 1],
                                              op=ALU.add), [sml], [sml])
            dvo(lambda e: e.tensor_tensor(out=base_, in0=endt, in1=ntf, op=ALU.subtract), [sml], [sml])
            dvo(lambda e: e.tensor_scalar(out=base_, in0=base_, scalar1=128.0, scalar2=None, op0=ALU.mult),
                [sml], [sml])
            dvo(lambda e: e.tensor_tensor(out=posf.ap.rearrange("p t e -> p (t e)"), in0=PS[0].ap[:, 0:NT * NE],
                                          in1=cum.ap[:, 0:NT, :].rearrange("p t e -> p (t e)"), op=ALU.add),
                [PS[0], cum], [posf])
            dvo(lambda e: e.tensor_tensor(out=posf.ap, in0=posf.ap,
                                          in1=base_.unsqueeze(1).broadcast_to([128, NT, NE]), op=ALU.add),
                [posf, sml], [posf])
            for si, oh_ in enumerate((oha, ohb)):
                dvo(lambda e: e.tensor_tensor(out=tmpf.ap, in0=posf.ap, in1=oh_.ap, op=ALU.mult),
                    [posf, oh_], [tmpf])
                dvo(lambda e: e.tensor_reduce(out=posa.ap[:, si, :], in_=tmpf.ap, axis=AX.X, op=ALU.add),
                    [tmpf], [posa])
            dvo(lambda e: e.tensor_copy(out=posi.ap, in_=posa.ap), [posa], [posi])
            dvo(lambda e: e.tensor_tensor(out=cmp3.ap, in0=iota.ap.unsqueeze(2).broadcast_to([128, NSLOT, NE]),
                                          in1=endt.unsqueeze(1).broadcast_to([128, NSLOT, NE]), op=ALU.is_ge),
                [iota, sml], [cmp3])
            dvo(lambda e: e.tensor_reduce(out=ejf.ap, in_=cmp3.ap, axis=AX.X, op=ALU.add), [cmp3], [ejf])
            if SKIP_UNUSED:
                dvo(lambda e: e.tensor_scalar(out=ejf.ap, in0=ejf.ap, scalar1=128.0, scalar2=None,
                                              op0=ALU.mult), [ejf], [ejf])
            else:
                dvo(lambda e: e.tensor_scalar(out=ejf.ap, in0=ejf.ap, scalar1=float(NE - 1), scalar2=128.0,
                                              op0=ALU.min, op1=ALU.mult), [ejf], [ejf])
            dvo(lambda e: e.tensor_scalar(out=ejf.ap, in0=ejf.ap, scalar1=pidx, scalar2=None, op0=ALU.add),
                [ejf, cf], [ejf])
            dvo(lambda e: e.tensor_copy(out=widx.ap, in_=ejf.ap), [ejf], [widx])
            if stage == "S1":
                raise _Stop()
            xs_res = Res("xs")
            for tt in range(NT):
                for si in range(2):
                    K.idma(xs_d, posi.ap[:, si, tt:tt + 1], xall.ap[:, tt, :], None, xall, None,
                           reads=[xall, posi])
            K.barrier()
            A.top = top_save
            sp_mark = A.mark()
            if stage == "S2":
                raise _Stop()
            NB_ = 3
            NBD = 5
            wgs = [A.alloc("wgs%d" % i, [128, 8, 512], BF16) for i in range(NB_)]
            wus = [A.alloc("wus%d" % i, [128, 8, 512], BF16) for i in range(NB_)]
            wds = [A.alloc("wds%d" % i, [128, 4, 1024], BF16) for i in range(NBD)]
            xst = [A.alloc("xst%d" % i, [128, D], BF16) for i in range(NB_)]
            xsT = [A.alloc("xsT%d" % i, [128, 8, 128], BF16) for i in range(NB_)]
            sgs = [A.alloc("sgs%d" % i, [128, 512], F32) for i in range(NB_)]
            hds = [A.alloc("hds%d" % i, [128, 4, 128], BF16) for i in range(NB_)]
            ysb = [A.alloc("ysb%d" % i, [128, D], F32) for i in range(NB_)]
            ys_res = Res("ys")

            def m_load(j):
                k = j % NB_
                K.dma("sp", [(xst[k].ap, xs_d[j * 128:(j + 1) * 128, :])], xst[k], writes=[xst[k]])
                for t_, d_, a_ in zip((wgs[k], wus[k], wds[j % NBD]), (wgb_d, wub_d, wdb_d), (8, 8, 4)):
                    K.idma(t_.ap.rearrange("p a b -> p (a b)"), None, d_, widx.ap[:, j:j + 1], t_,
                           wbound, reads=[widx], writes=[t_])

            def m_tr(j):
                k = j % NB_
                bk = j % 2
                for kc in range(8):
                    transpose_to(bk, kc * 128, xst[k].ap[:, kc * 128:(kc + 1) * 128], [xst[k]], kc == 7)

            def m_cp(j):
                k = j % NB_
                copy_any(xsT[k].ap, psb(j % 2).rearrange("p (a b) -> p a b", a=8), [PS[j % 2]], [xsT[k]])

            def m_gu(j):
                k = j % NB_
                wg_, wu_ = wgs[k], wus[k]
                for w_, bk in ((wg_, 2 + j % 2), (wu_, 4 + j % 2)):
                    for fc in range(4):
                        for kc in range(8):
                            mm(PS[bk].ap[:, fc * 128:(fc + 1) * 128], w_.ap[:, kc, fc * 128:(fc + 1) * 128],
                               xsT[k].ap[:, kc, :], kc == 0, kc == 7, [w_, xsT[k]], PS[bk],
                               fc == 3 and kc == 7)

            def m_act(j):
                k = j % NB_
                K.op("act", lambda e: e.activation(out=sgs[k].ap, in_=PS[2 + j % 2].ap, func=AF.Silu),
                     [PS[2 + j % 2]], [sgs[k]])
                K.op("dve", lambda e: e.tensor_tensor(out=hds[k].ap.rearrange("p a b -> p (a b)"),
                                                      in0=PS[4 + j % 2].ap, in1=sgs[k].ap, op=ALU.mult),
                     [PS[4 + j % 2], sgs[k]], [hds[k]])

            def m_dn(j):
                k = j % NB_
                wd_ = wds[j % NBD]
                for dh in range(2):
                    for fc in range(4):
                        mm(PS[6 + dh].ap, hds[k].ap[:, fc, :], wd_.ap[:, fc, dh * 512:(dh + 1) * 512],
                           fc == 0, fc == 3, [hds[k], wd_], PS[6 + dh], fc == 3)

            def m_ev(j):
                k = j % NB_
                K.op("act", lambda e: e.copy(out=ysb[k].ap[:, 0:512], in_=PS[6].ap), [PS[6]], [ysb[k]])
                K.op("dve", lambda e: e.tensor_copy(out=ysb[k].ap[:, 512:1024], in_=PS[7].ap), [PS[7]], [ysb[k]])
                K.dma("sp", [(ys_d[j * 128:(j + 1) * 128, :], ysb[k].ap)], ysb[k], reads=[ysb[k]],
                      writes=[ys_res])

            m_load(0)
            stages = [(m_ev, 4), (m_dn, 3), (m_act, 2), (m_cp, 1), (m_gu, 1), (m_tr, 0)]
            if stage == "S3a":
                stages = []
            elif stage == "S3b":
                stages = stages[3:]
            elif stage == "S3c":
                stages = stages[2:]
            nsl = 6 if stage in ("S3a", "S3b", "S3c") else NSLOT
            for i in range(nsl + 4):
                for fn_, d_ in stages:
                    j_ = i - d_
                    if 0 <= j_ < nsl:
                        fn_(j_)
                if i + 1 < nsl:
                    m_load(i + 1)
            if stage in ("S3a", "S3b", "S3c"):
                K.barrier()
                raise _Stop()
            K.barrier()
            if stage == "S3":
                raise _Stop()
            A.release(sp_mark)
            xr2 = [A.alloc("xr2_%d" % i, [128, D], F32) for i in range(5)]
            yga = [A.alloc("yga%d" % i, [128, D], F32) for i in range(3)]
            ygb = [A.alloc("ygb%d" % i, [128, D], F32) for i in range(3)]
            tmp2 = [A.alloc("tmp2_%d" % i, [128, D], F32) for i in range(2)]
            ot = [A.alloc("ot%d" % i, [128, D], F32) for i in range(3)]
            st2 = [A.alloc("st2_%d" % i, [128, 16], F32) for i in range(4)]

            def l_a(tt):
                j = tt % 3
                K.dma("sp", [(xr2[tt % 5].ap, x1s_d[b, tt * 128:(tt + 1) * 128, :])], xr2[tt % 5],
                      reads=[x1s_res[b][tt]], writes=[xr2[tt % 5]])
                K.idma(yga[j].ap, None, ys_d, posi.ap[:, 0, tt:tt + 1], yga[j], None,
                       reads=[posi, ys_res], writes=[yga[j]])
                K.idma(ygb[j].ap, None, ys_d, posi.ap[:, 1, tt:tt + 1], ygb[j], None,
                       reads=[posi, ys_res], writes=[ygb[j]])

            def l_b(tt):
                j = tt % 3
                K.op("dve", lambda e: e.scalar_tensor_tensor(out=xr2[tt % 5].ap, in0=yga[j].ap,
                                                             scalar=wts.ap[:, 0, tt:tt + 1], in1=xr2[tt % 5].ap,
                                                             op0=ALU.mult, op1=ALU.add), [yga[j], wts, xr2[tt % 5]],
                     [xr2[tt % 5]])
                K.op("dve", lambda e: e.scalar_tensor_tensor(out=xr2[tt % 5].ap, in0=ygb[j].ap,
                                                             scalar=wts.ap[:, 1, tt:tt + 1], in1=xr2[tt % 5].ap,
                                                             op0=ALU.mult, op1=ALU.add), [ygb[j], wts, xr2[tt % 5]],
                     [xr2[tt % 5]])
                ln_stats(xr2[tt % 5], st2[tt % 4])

            def l_c(tt):
                ln_rstd(st2[tt % 4])

            def l_d(tt):
                j = tt % 3
                ln_norm(xr2[tt % 5], 2, ot[j], tmp2[tt % 2], st2[tt % 4])
                K.dma("sp", [(out_d[b, tt * 128:(tt + 1) * 128, :], ot[j].ap)], ot[j], reads=[ot[j]])

            stages = [(l_a, -1), (l_d, 3), (l_c, 2), (l_b, 1)]
            l_a(0)
            for i in range(NT + 3):
                for fn_, d_ in stages:
                    t2_ = i - d_
                    if 0 <= t2_ < NT and not (fn_ is l_a and t2_ == 0):
                        fn_(t2_)

        for b in range(nb):
          try:
            K.barrier()
            A.release(base_mark)
            A.top = ARENA_BYTES
            xT = A.alloc("xT", [128, 8, S], BF16)
            wring = [A.alloc("wring%d" % i, [128, 8, 128], BF16) for i in range(8)]
            oT = A.alloc("oT", [128, 4, S], BF16)
            cnT = A.alloc("cnT", [128, 4, S], BF16)
            omT = A.alloc("omT", [128, 4, S], BF16)
            o_mark = A.mark()
            worder = []
            for it_ in range(12):
                for kind_ in range(3):
                    worder.append(kind_ * 12 + (it_ % 3) * 4 + it_ // 3)
            for ch_ in range(4):
                worder += [36 + ch_, 40 + ch_]
            worder += [44, 45, 46, 47]
            for dc_ in range(8):
                worder += [48 + dc_, 56 + dc_, 64 + dc_]
            wstate = {"issued": 0, "used": 0}
            LOOK = 5

            def _issue_w():
                i = wstate["issued"]
                if i < len(worder):
                    w = wring[i % 8]
                    K.dma("pool", [(w.ap, win_d[worder[i]])], w, writes=[w])
                    wstate["issued"] += 1

            def nextW(c):
                i = wstate["used"]
                assert worder[i] == c, (i, worder[i], c)
                while wstate["issued"] < min(len(worder), i + 1 + LOOK):
                    _issue_w()
                wstate["used"] += 1
                return wring[i % 8]

            getW = nextW

            kmT = A.alloc("kmT", [128, 4, MEM], BF16)
            vm = A.alloc("vm", [128, 2, 512], BF16)
            pa_mark = A.mark()
            cosT = A.alloc("cosT", [128, S], F32)
            sinS = A.alloc("sinS", [128, S], F32)
            tb_mark = A.mark()
            posI = A.alloc("posI", [128, S], I32)
            ang = A.alloc("ang", [128, S], F32)
            tq = A.alloc("tq", [128, S], F32)
            ki = A.alloc("ki", [128, S], I32)
            K.dma("sp", [(posI.ap, pos_d[b:b + 1, :].broadcast_to([128, S]))], posI, writes=[posI])
            K.op("dve", lambda e: e.tensor_copy(out=ang.ap, in_=posI.ap), [posI], [ang])
            K.op("dve", lambda e: e.tensor_scalar(out=ang.ap, in0=ang.ap, scalar1=invf, scalar2=None,
                                                  op0=ALU.mult), [ang, cf], [ang])
            for tab, shift in ((cosT, math.pi / 2), (sinS, 0.0)):
                K.op("dve", lambda e: e.tensor_scalar(out=tq.ap, in0=ang.ap, scalar1=1.0 / TWO_PI,
                                                      scalar2=shift / TWO_PI, op0=ALU.mult, op1=ALU.add),
                     [ang], [tq])
                K.op("dve", lambda e: e.tensor_copy(out=ki.ap, in_=tq.ap), [tq], [ki])
                K.op("dve", lambda e: e.tensor_copy(out=tq.ap, in_=ki.ap), [ki], [tq])
                K.op("dve", lambda e: e.scalar_tensor_tensor(out=tab.ap, in0=tq.ap, scalar=-C1, in1=ang.ap,
                                                             op0=ALU.mult, op1=ALU.add), [tq, ang], [tab])
                K.op("dve", lambda e: e.scalar_tensor_tensor(out=tab.ap, in0=tq.ap, scalar=-C2, in1=tab.ap,
                                                             op0=ALU.mult, op1=ALU.add), [tq, tab], [tab])
                hi = math.pi - shift - 1e-5
                lo = -math.pi - shift + 1e-5
                K.op("dve", lambda e: e.tensor_scalar(out=tab.ap, in0=tab.ap, scalar1=hi, scalar2=lo,
                                                      op0=ALU.min, op1=ALU.max), [tab], [tab])
                if shift == 0.0:
                    K.op("act", lambda e: e.activation(out=tab.ap, in_=tab.ap, func=AF.Sin), [tab], [tab])
                else:
                    K.op("dve", lambda e: e.tensor_scalar(out=tab.ap, in0=tab.ap, scalar1=shift, scalar2=None,
                                                          op0=ALU.add), [tab], [tab])
                    K.op("act", lambda e: e.activation(out=tab.ap, in_=tab.ap, func=AF.Sin), [tab], [tab])
            K.op("dve", lambda e: e.tensor_scalar(out=sinS.ap, in0=sinS.ap, scalar1=sgn, scalar2=None,
                                                  op0=ALU.mult), [sinS, cf], [sinS])
            xb = [A.alloc("xb%d" % i, [128, D], BF16) for i in range(4)]
            memT = A.alloc("memT", [128, 8, MEM], BF16)
            wkv = A.alloc("wkv", [128, 8, 1024], BF16)
            for t0_ in range(3):
                K.dma("pool", [(xb[t0_].ap, x_d[b, t0_ * 128:(t0_ + 1) * 128, :])], xb[t0_], writes=[xb[t0_]])
            K.dma("pool", [(wkv.ap, wkv_d)], wkv, writes=[wkv])
            for tt in range(NT):
                t_ = xb[tt % 4]
                if tt + 3 < NT:
                    tn_ = xb[(tt + 3) % 4]
                    K.dma("pool", [(tn_.ap, x_d[b, (tt + 3) * 128:(tt + 4) * 128, :])], tn_, writes=[tn_])
                bk = tt % 2
                for kc in range(8):
                    transpose_to(bk, kc * 128, t_.ap[:, kc * 128:(kc + 1) * 128], [t_], kc == 7)
                copy_any(xT.ap[:, :, tt * 128:(tt + 1) * 128],
                         psb(bk).rearrange("p (a b) -> p a b", a=8), [PS[bk]], [xT])
            for tt in range(2):
                t_ = xb[tt % 2]
                K.dma("pool", [(t_.ap, mem_d[b, tt * 128:(tt + 1) * 128, :])], t_, writes=[t_])
                bk = 2 + tt
                for kc in range(8):
                    transpose_to(bk, kc * 128, t_.ap[:, kc * 128:(kc + 1) * 128], [t_], kc == 7)
                copy_any(memT.ap[:, :, tt * 128:(tt + 1) * 128],
                         psb(bk).rearrange("p (a b) -> p a b", a=8), [PS[bk]], [memT])
            for hm in range(4):
                bk = 4 + hm % 2
                for kc in range(8):
                    mm(PS[bk].ap[:, 0:MEM], wkv.ap[:, kc, hm * 128:(hm + 1) * 128], memT.ap[:, kc, :],
                       kc == 0, kc == 7, [wkv, memT], PS[bk], kc == 7)
                copy_any(kmT.ap[:, hm, :], PS[bk].ap[:, 0:MEM], [PS[bk]], [kmT])
            for mc in range(2):
                bk = 6 + mc
                for kc in range(8):
                    mm(PS[bk].ap, memT.ap[:, kc, mc * 128:(mc + 1) * 128], wkv.ap[:, kc, 512:1024],
                       kc == 0, kc == 7, [wkv, memT], PS[bk], kc == 7)
                copy_any(vm.ap[:, mc, :], PS[bk].ap, [PS[bk]], [vm])
            if stage == "B":
                raise _Stop()
            K.barrier()
            A.release(tb_mark)

            if stage == "T":
                raise _Stop()
            accO = A.alloc("accO", [128, S], F32)
            accD = A.alloc("accD", [128, S], F32)
            qkv = [[A.alloc("qkv%d_%d" % (i, j), [128, S], BF16) for j in range(3)] for i in range(2)]
            Vp = [A.alloc("Vp%d" % i, [128, 16, 128], BF16) for i in range(2)]
            qb_t = [A.alloc("qb%d" % i, [128, 512], BF16) for i in range(2)]
            t1_t = [A.alloc("t1_%d" % i, [128, 512], F32) for i in range(2)]
            t2_t = [A.alloc("t2_%d" % i, [128, 512], F32) for i in range(2)]
            pT_t = [A.alloc("pT%d" % i, [128, 512], BF16) for i in range(2)]
            cnt = {"proj": 0, "rot": 0, "st": 0, "od": 0, "gi": 0}

            def sub_view(ap2d, r, tb):
                if r == 1:
                    return ap2d[:, tb * 512:(tb + 1) * 512]
                L = S // r
                v = ap2d.rearrange("p (r u) -> p r u", r=r)
                return v[:, :, tb * (512 // r):(tb + 1) * (512 // r)]

            def nat_view(ap2d, r):
                if r == 1:
                    return ap2d
                return ap2d.rearrange("p (u r) -> p r u", r=r)

            def c_stage0(blk):
                if blk["tb"] == 0:
                    blk["Wd"]["W"] = nextW(blk["c"])
                W = blk["Wd"]["W"]
                bk = cnt["proj"] % 2
                cnt["proj"] += 1
                blk["bk"] = bk
                tb = blk["tb"]
                for kc in range(8):
                    mm(PS[bk].ap, W.ap[:, kc, :], xT.ap[:, kc, tb * 512:(tb + 1) * 512],
                       kc == 0, kc == 7, [W, xT], PS[bk], kc == 7)

            def c_stage1(blk):
                bk, tb, c, r, dst = blk["bk"], blk["tb"], blk["c"], blk["r"], blk["dst"]
                if blk["kind"] == 2:
                    K.op("act", lambda e: e.activation(
                        out=sub_view(dst.ap, r, tb), in_=nat_view(PS[bk].ap, r), func=AF.Identity,
                        bias=binT.ap[:, c:c + 1], scale=1.0), [PS[bk], binT], [dst])
                    return
                j = cnt["rot"] % 2
                cnt["rot"] += 1
                qb, t1, t2 = qb_t[j], t1_t[j], t2_t[j]
                K.op("act", lambda e: e.activation(out=qb.ap, in_=PS[bk].ap, func=AF.Identity,
                                                   bias=binT.ap[:, c:c + 1], scale=1.0),
                     [PS[bk], binT], [qb])
                blk["rot"] = (j, qb, t1, t2)

            def c_stage2(blk):
                if blk["kind"] == 2:
                    return
                j, qb, t1, t2 = blk["rot"]
                tb, r, dst = blk["tb"], blk["r"], blk["dst"]
                rb = 2 + j
                mm(PS[rb].ap, perm, qb.ap, True, True, [cbf, qb], PS[rb], True)
                K.op("dve", lambda e: e.tensor_tensor(out=t1.ap, in0=qb.ap,
                                                      in1=cosT.ap[:, tb * 512:(tb + 1) * 512],
                                                      op=ALU.mult), [qb, cosT], [t1])
                K.op("dve", lambda e: e.tensor_tensor(out=t2.ap, in0=PS[rb].ap,
                                                      in1=sinS.ap[:, tb * 512:(tb + 1) * 512],
                                                      op=ALU.mult), [PS[rb], sinS], [t2])
                K.op("pool", lambda e: e.tensor_tensor(out=sub_view(dst.ap, r, tb),
                                                       in0=nat_view(t1.ap, r),
                                                       in1=nat_view(t2.ap, r), op=ALU.add),
                     [t1, t2], [dst])

            def c_proj(it):
                issue_precast(4)
                h, g = it // 3, it % 3
                r = DIL[g]
                blks = []
                for kind in range(3):
                    Wd = {}
                    for tb in range(NTB):
                        blks.append({"kind": kind, "c": kind * 12 + g * 4 + h, "tb": tb, "r": r,
                                     "dst": qkv[it % 2][kind], "Wd": Wd})
                n = len(blks)
                for i in range(n + 2):
                    if i < n:
                        c_stage0(blks[i])
                    if 1 <= i <= n:
                        c_stage1(blks[i - 1])
                    if i >= 2:
                        c_stage2(blks[i - 2])

            def c_attn(it):
                h, g = it // 3, it % 3
                r = DIL[g]
                L = S // r
                nbk = L // 128
                qT_, kT_, vT_ = qkv[it % 2]
                vp = Vp[it % 2]
                for half in range(2):
                    bk = 2 + half
                    for jj in range(8):
                        jb = half * 8 + jj
                        transpose_to(bk, jj * 128, vT_.ap[:, jb * 128:(jb + 1) * 128], [vT_], jj == 7)
                    copy_any(vp.ap[:, half * 8:(half + 1) * 8, :],
                             psb(bk).rearrange("p (a b) -> p a b", a=8), [PS[bk]], [vp])

                def s_stage(pr):
                    sb = 4 + pr % 2
                    for bi, jb in enumerate((2 * pr, 2 * pr + 1)):
                        n = jb % nbk
                        jp = jb - 1 if n > 0 else jb
                        mk = m_cp if n > 0 else m_ca
                        qs = qT_.ap[:, jb * 128:(jb + 1) * 128]
                        c0 = bi * 256
                        mm(PS[sb].ap[:, c0:c0 + 128], kT_.ap[:, jb * 128:(jb + 1) * 128], qs,
                           bi == 0, False, [kT_, qT_], PS[sb], False, skip=True)
                        mm(PS[sb].ap[:, c0 + 128:c0 + 256], kT_.ap[:, jp * 128:(jp + 1) * 128], qs,
                           False, False, [kT_, qT_], PS[sb], False, skip=True)
                        mm(PS[sb].ap[:, c0:c0 + 256], ident, mk, False, True, [cbf], PS[sb], bi == 1, skip=True)

                def e_stage(pr):
                    sb = 4 + pr % 2
                    pT = pT_t[pr % 2]
                    K.op("act", lambda e: e.activation(out=pT.ap, in_=PS[sb].ap, func=AF.Exp, scale=SCALE),
                         [PS[sb]], [pT])

                def p_stage(pr):
                    ob = 6 + pr % 2
                    pT = pT_t[pr % 2]
                    blocks = (2 * pr, 2 * pr + 1)
                    for bi, jb in enumerate(blocks):
                        n = jb % nbk
                        jp = jb - 1 if n > 0 else jb
                        c0 = bi * 256
                        o_ap = PS[ob].ap[:, bi * 128:(bi + 1) * 128]
                        d_ap = PS[ob].ap[:, 256 + bi * 128:256 + (bi + 1) * 128]
                        mm(o_ap, vp.ap[:, jb, :], pT.ap[:, c0:c0 + 128], bi == 0, False, [vp, pT], PS[ob], False,
                           skip=True)
                        mm(o_ap, vp.ap[:, jp, :], pT.ap[:, c0 + 128:c0 + 256], False, True, [vp, pT],
                           PS[ob], False, skip=True)

                    pT3 = pT.ap.rearrange("p (b c q) -> p b c q", b=2, c=2)
                    d2 = PS[ob].ap[:, 256:512].rearrange("p (b q) -> p b q", b=2)
                    mm(d2, onesb, pT3[:, :, 0, :], False, False, [cbf, pT], PS[ob], False, skip=True)
                    mm(d2, onesb, pT3[:, :, 1, :], False, True, [cbf, pT], PS[ob], True, skip=True)
                    jb0 = blocks[0]
                    if nbk >= 2:
                        res_, n0 = jb0 // nbk, jb0 % nbk
                        if r == 1:
                            dO = accO.ap[:, n0 * 128:n0 * 128 + 256]
                            dD = accD.ap[:, n0 * 128:n0 * 128 + 256]
                        else:
                            dO = accO.ap.rearrange("p (u r) -> p r u", r=r)[:, res_, n0 * 128:n0 * 128 + 256]
                            dD = accD.ap.rearrange("p (u r) -> p r u", r=r)[:, res_, n0 * 128:n0 * 128 + 256]
                        sO = PS[ob].ap[:, 0:256]
                        sD = PS[ob].ap[:, 256:512]
                    else:
                        dO = accO.ap.rearrange("p (u r) -> p r u", r=r)[:, jb0:jb0 + 2, :]
                        dD = accD.ap.rearrange("p (u r) -> p r u", r=r)[:, jb0:jb0 + 2, :]
                        sO = PS[ob].ap[:, 0:256].rearrange("p (a b) -> p a b", a=2)
                        sD = PS[ob].ap[:, 256:512].rearrange("p (a b) -> p a b", a=2)
                    if g == 0:
                        K.op("act", lambda e: e.copy(out=dO, in_=sO), [PS[ob]], [accO])
                        K.op("act", lambda e: e.copy(out=dD, in_=sD), [PS[ob]], [accD])
                    else:
                        K.op("dve", lambda e: e.tensor_tensor(out=dO, in0=sO, in1=dO, op=ALU.add),
                             [PS[ob], accO], [accO])
                        K.op("dve", lambda e: e.tensor_tensor(out=dD, in0=sD, in1=dD, op=ALU.add),
                             [PS[ob], accD], [accD])
                    if g == 2:
                        oV = oT.ap[:, h, :].rearrange("p (u r) -> p r u", r=r)[:, jb0:jb0 + 2, :]
                        K.op("dve", lambda e: e.reciprocal(out=dD, in_=dD), [accD], [accD])
                        K.op("pool", lambda e: e.tensor_tensor(out=oV, in0=dO, in1=dD, op=ALU.mult),
                             [accO, accD], [oT])

                s_stage(0)
                for pr in range(8):
                    if pr + 1 < 8:
                        s_stage(pr + 1)
                    e_stage(pr)
                    p_stage(pr)

            NIT = 12
            c_proj(0)
            for it in range(NIT):
                if it + 1 < NIT:
                    c_proj(it + 1)
                c_attn(it)
            K.barrier()
            A.release(pa_mark)
            if stage == "C":
                raise _Stop()
            cT = A.alloc("cT", [128, 4, S + 32], BF16)
            dg = A.alloc("dg", [128, 4 * 31, 128], BF16)
            ga = [A.alloc("ga%d" % i, [128, 512], F32) for i in range(2)]
            gs = [A.alloc("gs%d" % i, [128, 512], F32) for i in range(2)]
            zf = A.alloc("zf", [128, 4, 512], F32)
            zq = A.alloc("zq", [128, 4, 512], F32)
            mS = A.alloc("mS", [128, 512], F32)
            rS = A.alloc("rS", [128, 512], F32)
            K.op("pool", lambda e: e.memset(cT.ap[:, :, 0:32], 0.0), [], [cT])
            dg_todo = [(ch, j) for ch in range(4) for j in range(31)]

            def build_dg(n):
                for _ in range(n):
                    if dg_todo:
                        ch_, j_ = dg_todo.pop(0)
                        K.op("dve", lambda e: e.tensor_scalar(out=dg.ap[:, ch_ * 31 + j_, :], in0=ident,
                                                              scalar1=convw.ap[:, ch_, j_:j_ + 1], scalar2=None,
                                                              op0=ALU.mult), [cbf, convw], [dg])
            for ch in range(4):
                Wa = getW(36 + ch)
                Wb = getW(40 + ch)
                for tb in range(NTB):
                    j = (ch * NTB + tb) % 2
                    for W, bk, dst, fn, c in ((Wa, 2 * j, ga[j], AF.Identity, 36 + ch),
                                              (Wb, 2 * j + 1, gs[j], AF.Sigmoid, 40 + ch)):
                        for kc in range(8):
                            mm(PS[bk].ap, W.ap[:, kc, :], xT.ap[:, kc, tb * 512:(tb + 1) * 512],
                               kc == 0, kc == 7, [W, xT], PS[bk], kc == 7)
                        K.op("act", lambda e: e.activation(out=dst.ap, in_=PS[bk].ap, func=fn,
                                                           bias=binT.ap[:, c:c + 1], scale=1.0),
                             [PS[bk], binT], [dst])
                    K.op("dve", lambda e: e.tensor_tensor(out=cT.ap[:, ch, 32 + tb * 512:32 + (tb + 1) * 512],
                                                          in0=ga[j].ap, in1=gs[j].ap, op=ALU.mult),
                         [ga[j], gs[j]], [cT])
                    build_dg(8)
            for tb in range(NTB):
                for ch in range(4):
                    bk = 2 + ch
                    for j in range(31):
                        c0 = 2 + tb * 512 + j
                        mm(PS[bk].ap, dg.ap[:, ch * 31 + j, :], cT.ap[:, ch, c0:c0 + 512],
                           j == 0, j == 30, [dg, cT], PS[bk], j == 30)
                    K.op("act", lambda e: e.activation(out=zf.ap[:, ch, :], in_=PS[bk].ap, func=AF.Identity,
                                                       bias=convp.ap[:, 0, ch:ch + 1], scale=1.0),
                         [PS[bk], convp], [zf])
                    K.op("act", lambda e: e.activation(out=zq.ap[:, ch, :], in_=PS[bk].ap, func=AF.Square,
                                                       bias=convp.ap[:, 0, ch:ch + 1], scale=1.0),
                         [PS[bk], convp], [zq])
                for ch in range(4):
                    mm(PS[6].ap, onesf.ap, zf.ap[:, ch, :], ch == 0, ch == 3, [onesf, zf], PS[6], ch == 3)
                for ch in range(4):
                    mm(PS[7].ap, onesf.ap, zq.ap[:, ch, :], ch == 0, ch == 3, [onesf, zq], PS[7], ch == 3)
                K.op("act", lambda e: e.copy(out=mS.ap, in_=PS[6].ap), [PS[6]], [mS])
                K.op("dve", lambda e: e.tensor_tensor(out=rS.ap, in0=mS.ap, in1=mS.ap, op=ALU.mult), [mS], [rS])
                K.op("dve", lambda e: e.tensor_tensor(out=rS.ap, in0=PS[7].ap, in1=rS.ap, op=ALU.subtract),
                     [PS[7], rS], [rS])
                K.op("act", lambda e: e.activation(out=rS.ap, in_=rS.ap, func=AF.Sqrt, bias=EPS, scale=1.0),
                     [rS], [rS])
                K.op("dve", lambda e: e.reciprocal(out=rS.ap, in_=rS.ap), [rS], [rS])
                for ch in range(4):
                    K.op("dve", lambda e: e.tensor_tensor(out=zf.ap[:, ch, :], in0=zf.ap[:, ch, :], in1=mS.ap,
                                                          op=ALU.subtract), [zf, mS], [zf])
                    K.op("pool", lambda e: e.tensor_tensor(out=zf.ap[:, ch, :], in0=zf.ap[:, ch, :], in1=rS.ap,
                                                           op=ALU.mult), [zf, rS], [zf])
                    K.op("act", lambda e: e.activation(out=cnT.ap[:, ch, tb * 512:(tb + 1) * 512],
                                                       in_=zf.ap[:, ch, :], func=AF.Silu,
                                                       bias=convp.ap[:, 2, ch:ch + 1],
                                                       scale=convp.ap[:, 1, ch:ch + 1]), [zf, convp], [cnT])
            K.barrier()
            A.release(pa_mark)
            if stage == "D":
                raise _Stop()
            qm_t = [A.alloc("qm%d" % i, [128, 512], BF16) for i in range(2)]
            pm_t = [A.alloc("pm%d" % i, [128, 2, 512], BF16) for i in range(2)]
            rd_t = [A.alloc("rd%d" % i, [128, 512], F32) for i in range(2)]
            Wm = {}

            def e_s0(q):
                hm, tb = q // NTB, q % NTB
                if tb == 0:
                    Wm[hm] = getW(44 + hm)
                W = Wm[hm]
                bk = q % 2
                for kc in range(8):
                    mm(PS[bk].ap, W.ap[:, kc, :], xT.ap[:, kc, tb * 512:(tb + 1) * 512],
                       kc == 0, kc == 7, [W, xT], PS[bk], kc == 7)

            def e_s1(q):
                hm = q // NTB
                j = q % 2
                qm = qm_t[j]
                c = 44 + hm
                K.op("act", lambda e: e.activation(out=qm.ap, in_=PS[j].ap, func=AF.Identity,
                                                   bias=binT.ap[:, c:c + 1], scale=1.0), [PS[j], binT], [qm])
                for mc in range(2):
                    sbk = 2 + 2 * j + mc
                    mm(PS[sbk].ap, kmT.ap[:, hm, mc * 128:(mc + 1) * 128], qm.ap, True, True, [kmT, qm],
                       PS[sbk], True)

            def e_s2(q):
                j = q % 2
                pm = pm_t[j]
                for mc in range(2):
                    sbk = 2 + 2 * j + mc
                    K.op("act", lambda e: e.activation(out=pm.ap[:, mc, :], in_=PS[sbk].ap, func=AF.Exp,
                                                       scale=SCALE), [PS[sbk]], [pm])

            def e_s3(q):
                hm = q // NTB
                pm = pm_t[q % 2]
                for mc in range(2):
                    mm(PS[6].ap, vm.ap[:, mc, hm * 128:(hm + 1) * 128], pm.ap[:, mc, :], mc == 0, mc == 1,
                       [vm, pm], PS[6], mc == 1)
                for mc in range(2):
                    mm(PS[7].ap, onesb, pm.ap[:, mc, :], mc == 0, mc == 1, [cbf, pm], PS[7], mc == 1)

            def e_s4(q):
                hm, tb = q // NTB, q % NTB
                rd = rd_t[q % 2]
                K.op("dve", lambda e: e.reciprocal(out=rd.ap, in_=PS[7].ap), [PS[7]], [rd])
                K.op("dve", lambda e: e.tensor_tensor(out=omT.ap[:, hm, tb * 512:(tb + 1) * 512],
                                                      in0=PS[6].ap, in1=rd.ap, op=ALU.mult),
                     [PS[6], rd], [omT])

            NQ = 4 * NTB
            stages = [(e_s4, 4), (e_s3, 3), (e_s2, 2), (e_s1, 1), (e_s0, 0)]
            for i in range(NQ + 4):
                for fn_, d_ in stages:
                    q_ = i - d_
                    if 0 <= q_ < NQ:
                        fn_(q_)
            K.barrier()
            A.release(o_mark)
            if stage == "E":
                raise _Stop()
            wo3 = [A.alloc("wo%d" % i, [128, 4, 1024], BF16) for i in range(3)]
            for w_, d_ in zip(wo3, (wao_d, wco_d, wmo_d)):
                K.dma("pool", [(w_.ap, d_)], w_, writes=[w_])
            mergedT0 = A.alloc("mergedT0", [128, 8, S], BF16)
            mergedT = mergedT0
            gt = [[A.alloc("gt%d_%d" % (i, j), [128, 512], F32) for j in range(3)] for i in range(2)]
            mt = [[A.alloc("mt%d_%d" % (i, j), [128, 512], F32) for j in range(3)] for i in range(2)]
            srcs = (oT, cnT, omT)
            it = 0
            pj = 0
            for dc in range(8):
                Ws = [getW(48 + br * 8 + dc) for br in range(3)]
                for tb in range(NTB):
                    j = it % 2
                    it += 1
                    for br in range(3):
                        bk = pj % 4
                        pj += 1
                        c = 48 + br * 8 + dc
                        for kc in range(8):
                            mm(PS[bk].ap, Ws[br].ap[:, kc, :], xT.ap[:, kc, tb * 512:(tb + 1) * 512],
                               kc == 0, kc == 7, [Ws[br], xT], PS[bk], kc == 7)
                        g_ = gt[j][br]
                        K.op("act", lambda e: e.activation(out=g_.ap, in_=PS[bk].ap, func=AF.Sigmoid,
                                                           bias=binT.ap[:, c:c + 1], scale=1.0),
                             [PS[bk], binT], [g_])
                        yb = 4 + (it * 3 + br) % 4
                        for hc in range(4):
                            mm(PS[yb].ap, wo3[br].ap[:, hc, dc * 128:(dc + 1) * 128],
                               srcs[br].ap[:, hc, tb * 512:(tb + 1) * 512], hc == 0, hc == 3,
                               [wo3[br], srcs[br]], PS[yb], hc == 3)
                        m_ = mt[j][br]
                        K.op("dve", lambda e: e.tensor_tensor(out=m_.ap, in0=PS[yb].ap, in1=g_.ap, op=ALU.mult),
                             [PS[yb], g_], [m_])
                    K.op("pool", lambda e: e.tensor_tensor(out=mt[j][0].ap, in0=mt[j][0].ap, in1=mt[j][1].ap,
                                                           op=ALU.add), [mt[j][0], mt[j][1]], [mt[j][0]])
                    K.op("pool", lambda e: e.tensor_tensor(out=mergedT.ap[:, dc, tb * 512:(tb + 1) * 512],
                                                           in0=mt[j][0].ap, in1=mt[j][2].ap, op=ALU.add),
                         [mt[j][0], mt[j][2]], [mergedT])
            K.barrier()
            A.release(base_mark)
            if stage == "F1":
                raise _Stop()
            mergedT = A.alloc("mergedT", [128, 8, S], BF16)
            wout = A.alloc("wout", [128, 8, 1024], BF16)
            K.dma("pool", [(wout.ap, wout_d)], wout, writes=[wout])
            K.op("dve", lambda e: e.tensor_copy(out=mergedT.ap[:, 0:4, :], in_=mergedT0.ap[:, 0:4, :]),
                 [mergedT0], [mergedT])
            K.op("act", lambda e: e.copy(out=mergedT.ap[:, 4:6, :], in_=mergedT0.ap[:, 4:6, :]),
                 [mergedT0], [mergedT])
            K.op("dve", lambda e: e.tensor_copy(out=mergedT.ap[:, 6:8, :], in_=mergedT0.ap[:, 6:8, :]),
                 [mergedT0], [mergedT])
            K.barrier()
            x1T = A.alloc("x1T", [128, 8, S], BF16, top=True)
            gate = A.alloc("gate", [128, NT, 16], F32, top=True)
            oha = A.alloc("oha", [128, NT, 16], F32, top=True)
            ohb = A.alloc("ohb", [128, NT, 16], F32, top=True)
            wts = A.alloc("wts", [128, 2, NT], F32, top=True)
            xr = [A.alloc("xr%d" % i, [128, D], F32) for i in range(4)]
            vt = [A.alloc("vt%d" % i, [128, D], F32) for i in range(4)]
            tmpn = [A.alloc("tmpn%d" % i, [128, D], F32) for i in range(4)]
            x1t = [A.alloc("x1t%d" % i, [128, D], F32) for i in range(4)]
            xhi = [A.alloc("xhi%d" % i, [128, D], BF16) for i in range(4)]
            xlo = [A.alloc("xlo%d" % i, [128, D], BF16) for i in range(4)]
            xloT = [A.alloc("xloT%d" % i, [128, 8, 128], BF16) for i in range(4)]
            stt = [A.alloc("st%d" % i, [128, 16], F32) for i in range(4)]
            lg = A.alloc("lg", [128, NT, 20], F32)
            def f2_ldx(tt):
                j = tt % 4
                K.dma("sp", [(xr[j].ap, x_d[b, tt * 128:(tt + 1) * 128, :])], xr[j], writes=[xr[j]])

            def f2_mix(tt):
                j = tt % 4
                for dh in range(2):
                    bk = 2 * (tt % 2) + dh
                    for kc in range(8):
                        mm(PS[bk].ap, mergedT.ap[:, kc, tt * 128:(tt + 1) * 128],
                           wout.ap[:, kc, dh * 512:(dh + 1) * 512], kc == 0, kc == 7, [mergedT, wout], PS[bk],
                           kc == 7)
                    K.op("dve", lambda e: e.scalar_tensor_tensor(
                        out=vt[j].ap[:, dh * 512:(dh + 1) * 512], in0=xr[j].ap[:, dh * 512:(dh + 1) * 512],
                        scalar=ALPHA, in1=PS[bk].ap, op0=ALU.mult, op1=ALU.add), [xr[j], PS[bk]], [vt[j]])

            def f2_s1(tt):
                ln_rstd(stt[tt % 4])

            def f2_s2(tt):
                j = tt % 4
                ln_norm(vt[j], 0, x1t[j], tmpn[j], stt[j])

            def f2_s3(tt):
                j = tt % 4
                K.op("act", lambda e: e.copy(out=xhi[j].ap, in_=x1t[j].ap), [x1t[j]], [xhi[j]])
                K.op("pool", lambda e: e.tensor_tensor(out=xlo[j].ap, in0=x1t[j].ap, in1=xhi[j].ap,
                                                       op=ALU.subtract), [x1t[j], xhi[j]], [xlo[j]])
                if SPARSE:
                    K.dma("sp", [(x1h_d[b, tt * 128:(tt + 1) * 128, :], xhi[j].ap)], xhi[j],
                          reads=[xhi[j]], writes=[x1h_res[b][tt]])
                K.op("act", lambda e: e.activation(out=tmpn[j].ap, in_=x1t[j].ap, func=AF.Copy, scale=ALPHA),
                     [x1t[j]], [tmpn[j]])
                K.dma("sp", [(x1s_d[b, tt * 128:(tt + 1) * 128, :], tmpn[j].ap)], tmpn[j],
                      reads=[tmpn[j]], writes=[x1s_res[b][tt]])

            def f2_s4(tt):
                j = tt % 4
                bh = 4 + tt % 2
                for kc in range(8):
                    transpose_to(bh, kc * 128, xhi[j].ap[:, kc * 128:(kc + 1) * 128], [xhi[j]], kc == 7)
                bl = 6
                for kc in range(8):
                    transpose_to(bl, kc * 128, xlo[j].ap[:, kc * 128:(kc + 1) * 128], [xlo[j]], kc == 7)

            def f2_s5(tt):
                j = tt % 4
                bh = 4 + tt % 2
                bl = 6
                K.op("act", lambda e: e.copy(out=x1T.ap[:, :, tt * 128:(tt + 1) * 128],
                                             in_=psb(bh).rearrange("p (a b) -> p a b", a=8)), [PS[bh]], [x1T])
                K.op("dve", lambda e: e.tensor_copy(out=xloT[j].ap,
                                                    in_=psb(bl).rearrange("p (a b) -> p a b", a=8)),
                     [PS[bl]], [xloT[j]])

            def f2_s6(tt):
                j = tt % 4
                terms = []
                for kc in range(8):
                    terms.append((x1T.ap[:, kc, tt * 128:(tt + 1) * 128], wr_hi.ap[:, kc, :]))
                    terms.append((xloT[j].ap[:, kc, :], wr_hi.ap[:, kc, :]))
                    terms.append((x1T.ap[:, kc, tt * 128:(tt + 1) * 128], wr_lo.ap[:, kc, :]))
                for ti, (l_, r_) in enumerate(terms):
                    mm(PS[7].ap[:, 0:20], l_, r_, ti == 0, ti == len(terms) - 1, [x1T, xloT[j], wr_hi, wr_lo],
                       PS[7], ti == len(terms) - 1)
                K.op("dve", lambda e: e.tensor_tensor(out=lg.ap[:, tt, :], in0=PS[7].ap[:, 0:20], in1=brb.ap,
                                                      op=ALU.add), [PS[7], brb], [lg])

            stages = [f2_s6, f2_s5, f2_s4, f2_s3, f2_s2, f2_s1]
            nst = len(stages)
            for i in range(NT + nst):
                for k, fn_ in enumerate(stages):
                    tt_ = i - (nst - k)
                    if 0 <= tt_ < NT:
                        fn_(tt_)
                if i == 0:
                    f2_ldx(0)
                    f2_ldx(1)
                if i + 2 < NT:
                    f2_ldx(i + 2)
                if i < NT:
                    f2_mix(i)
                    ln_stats(vt[i % 4], stt[i % 4])
            if stage == "F2":
                raise _Stop()
            r_ = {}
            for nm, shp in (("gmax", [128, NT]), ("ohg", [128, NT, 4]), ("dg_", [128, NT, 4]),
                            ("gsum", [128, NT]), ("tmp4", [128, NT, 4, 4]), ("els", [128, NT, 4]),
                            ("m1", [128, NT]), ("oh1", [128, NT, 4]), ("msk", [128, NT, 4]),
                            ("m2", [128, NT]), ("oh2", [128, NT, 4]), ("e2", [128, NT]), ("w1", [128, NT]),
                            ("w2", [128, NT]), ("ew", [128, NT, 4]), ("ew2", [128, NT, 4])):
                r_[nm] = A.alloc("r_" + nm, shp, F32)
            gl = lg.ap[:, :, 0:4]
            el = lg.ap[:, :, 4:20].rearrange("p t (g e) -> p t g e", g=4)

            def bc3(t2):
                return t2.unsqueeze(2).broadcast_to([128, NT, 4])

            def dv(fn, reads, writes):
                K.op("dve", fn, reads, writes)
            R = r_
            dv(lambda e: e.tensor_reduce(out=R["gmax"].ap, in_=gl, axis=AX.X, op=ALU.max), [lg], [R["gmax"]])
            dv(lambda e: e.tensor_tensor(out=R["ohg"].ap, in0=gl, in1=bc3(R["gmax"].ap), op=ALU.is_equal),
               [lg, R["gmax"]], [R["ohg"]])
            dv(lambda e: e.tensor_tensor(out=R["dg_"].ap, in0=gl, in1=bc3(R["gmax"].ap), op=ALU.subtract),
               [lg, R["gmax"]], [R["dg_"]])
            K.op("act", lambda e: e.activation(out=R["dg_"].ap, in_=R["dg_"].ap, func=AF.Exp), [R["dg_"]],
                 [R["dg_"]])
            dv(lambda e: e.tensor_reduce(out=R["gsum"].ap, in_=R["dg_"].ap, axis=AX.X, op=ALU.add), [R["dg_"]],
               [R["gsum"]])
            dv(lambda e: e.reciprocal(out=R["gsum"].ap, in_=R["gsum"].ap), [R["gsum"]], [R["gsum"]])
            dv(lambda e: e.tensor_tensor(out=R["tmp4"].ap, in0=el,
                                         in1=R["ohg"].ap.unsqueeze(3).broadcast_to([128, NT, 4, 4]),
                                         op=ALU.mult), [lg, R["ohg"]], [R["tmp4"]])
            dv(lambda e: e.tensor_reduce(out=R["els"].ap, in_=R["tmp4"].ap.rearrange("p t g e -> p t e g"),
                                         axis=AX.X, op=ALU.add), [R["tmp4"]], [R["els"]])
            dv(lambda e: e.tensor_reduce(out=R["m1"].ap, in_=R["els"].ap, axis=AX.X, op=ALU.max), [R["els"]],
               [R["m1"]])
            dv(lambda e: e.tensor_tensor(out=R["oh1"].ap, in0=R["els"].ap, in1=bc3(R["m1"].ap), op=ALU.is_equal),
               [R["els"], R["m1"]], [R["oh1"]])
            dv(lambda e: e.scalar_tensor_tensor(out=R["msk"].ap, in0=R["oh1"].ap, scalar=-1e30, in1=R["els"].ap,
                                                op0=ALU.mult, op1=ALU.add), [R["oh1"], R["els"]], [R["msk"]])
            dv(lambda e: e.tensor_reduce(out=R["m2"].ap, in_=R["msk"].ap, axis=AX.X, op=ALU.max), [R["msk"]],
               [R["m2"]])
            dv(lambda e: e.tensor_tensor(out=R["oh2"].ap, in0=R["msk"].ap, in1=bc3(R["m2"].ap), op=ALU.is_equal),
               [R["msk"], R["m2"]], [R["oh2"]])
            dv(lambda e: e.tensor_tensor(out=R["e2"].ap, in0=R["m2"].ap, in1=R["m1"].ap, op=ALU.subtract),
               [R["m2"], R["m1"]], [R["e2"]])
            K.op("act", lambda e: e.activation(out=R["e2"].ap, in_=R["e2"].ap, func=AF.Exp), [R["e2"]], [R["e2"]])
            dv(lambda e: e.tensor_scalar(out=R["w1"].ap, in0=R["e2"].ap, scalar1=1.0, scalar2=None, op0=ALU.add),
               [R["e2"]], [R["w1"]])
            dv(lambda e: e.reciprocal(out=R["w1"].ap, in_=R["w1"].ap), [R["w1"]], [R["w1"]])
            dv(lambda e: e.tensor_tensor(out=R["w2"].ap, in0=R["e2"].ap, in1=R["w1"].ap, op=ALU.mult),
               [R["e2"], R["w1"]], [R["w2"]])
            dv(lambda e: e.tensor_tensor(out=R["w1"].ap, in0=R["w1"].ap, in1=R["gsum"].ap, op=ALU.mult),
               [R["w1"], R["gsum"]], [R["w1"]])
            dv(lambda e: e.tensor_tensor(out=R["w2"].ap, in0=R["w2"].ap, in1=R["gsum"].ap, op=ALU.mult),
               [R["w2"], R["gsum"]], [R["w2"]])
            if SPARSE:
                dv(lambda e: e.tensor_copy(out=wts.ap[:, 0, :], in_=R["w1"].ap), [R["w1"]], [wts])
                dv(lambda e: e.tensor_copy(out=wts.ap[:, 1, :], in_=R["w2"].ap), [R["w2"]], [wts])
                for oh_, o_ in ((R["oh1"], oha), (R["oh2"], ohb)):
                    dv(lambda e: e.tensor_tensor(out=o_.ap.rearrange("p t (g e) -> p t g e", g=4),
                                                 in0=R["ohg"].ap.unsqueeze(3).broadcast_to([128, NT, 4, 4]),
                                                 in1=oh_.ap.unsqueeze(2).broadcast_to([128, NT, 4, 4]),
                                                 op=ALU.mult), [R["ohg"], oh_], [o_])
            dv(lambda e: e.tensor_tensor(out=R["ew"].ap, in0=R["oh1"].ap, in1=bc3(R["w1"].ap), op=ALU.mult),
               [R["oh1"], R["w1"]], [R["ew"]])
            dv(lambda e: e.tensor_tensor(out=R["ew2"].ap, in0=R["oh2"].ap, in1=bc3(R["w2"].ap), op=ALU.mult),
               [R["oh2"], R["w2"]], [R["ew2"]])
            dv(lambda e: e.tensor_tensor(out=R["ew"].ap, in0=R["ew"].ap, in1=R["ew2"].ap, op=ALU.add),
               [R["ew"], R["ew2"]], [R["ew"]])
            dv(lambda e: e.tensor_tensor(out=gate.ap.rearrange("p t (g e) -> p t g e", g=4),
                                         in0=R["ohg"].ap.unsqueeze(3).broadcast_to([128, NT, 4, 4]),
                                         in1=R["ew"].ap.unsqueeze(2).broadcast_to([128, NT, 4, 4]),
                                         op=ALU.mult), [R["ohg"], R["ew"]], [gate])
            K.barrier()
            A.release(base_mark)
            if stage == "R":
                raise _Stop()
            if SPARSE:
                sparse_moe(b, oha, ohb, wts)
                continue
            yacc = A.alloc("yacc", [128, NT, D], F32)
            moe_w_mark = A.mark()
            ew_ = [[A.alloc("wg%d" % i, [128, 8, 512], BF16), A.alloc("wu%d" % i, [128, 8, 512], BF16),
                    A.alloc("wd%d" % i, [128, 4, 1024], BF16)] for i in range(2)]
            hid = [A.alloc("hid%d" % i, [128, 4, 512], BF16) for i in range(2)]
            sg = [A.alloc("sg%d" % i, [128, 512], F32) for i in range(2)]

            def load_expert(e_):
                s_ = ew_[e_ % 2]
                for t_, d_ in zip(s_, (wg_d, wu_d, wd_d)):
                    K.dma("pool", [(t_.ap, d_[e_])], t_, writes=[t_])
            load_expert(0)
            gu = 0
            yb_ = 0
            for ex in range(16):
                if ex + 1 < 16:
                    load_expert(ex + 1)
                wg_, wu_, wd_ = ew_[ex % 2]
                for tb in range(NTB):
                    hd = hid[(ex * NTB + tb) % 2]
                    for fc in range(4):
                        bg = 2 * (gu % 2)
                        bu = bg + 1
                        gu += 1
                        for kc in range(8):
                            mm(PS[bg].ap, wg_.ap[:, kc, fc * 128:(fc + 1) * 128],
                               x1T.ap[:, kc, tb * 512:(tb + 1) * 512], kc == 0, kc == 7, [wg_, x1T], PS[bg],
                               kc == 7)
                        for kc in range(8):
                            mm(PS[bu].ap, wu_.ap[:, kc, fc * 128:(fc + 1) * 128],
                               x1T.ap[:, kc, tb * 512:(tb + 1) * 512], kc == 0, kc == 7, [wu_, x1T], PS[bu],
                               kc == 7)
                        s_ = sg[gu % 2]
                        K.op("act", lambda e: e.activation(out=s_.ap, in_=PS[bg].ap, func=AF.Silu), [PS[bg]], [s_])
                        K.op("dve", lambda e: e.tensor_tensor(out=hd.ap[:, fc, :], in0=PS[bu].ap, in1=s_.ap,
                                                              op=ALU.mult), [PS[bu], s_], [hd])
                    for ti in range(4):
                        tt = tb * 4 + ti
                        for dh in range(2):
                            by = 4 + (yb_ % 4)
                            yb_ += 1
                            for fc in range(4):
                                mm(PS[by].ap, hd.ap[:, fc, ti * 128:(ti + 1) * 128],
                                   wd_.ap[:, fc, dh * 512:(dh + 1) * 512], fc == 0, fc == 3, [hd, wd_], PS[by],
                                   fc == 3)
                            ya = yacc.ap[:, tt, dh * 512:(dh + 1) * 512]
                            gsc = gate.ap[:, tt, ex:ex + 1]
                            if ex == 0:
                                K.op("dve", lambda e: e.tensor_scalar(out=ya, in0=PS[by].ap, scalar1=gsc,
                                                                      scalar2=None, op0=ALU.mult),
                                     [PS[by], gate], [yacc])
                            else:
                                K.op("dve", lambda e: e.scalar_tensor_tensor(out=ya, in0=PS[by].ap, scalar=gsc,
                                                                             in1=ya, op0=ALU.mult, op1=ALU.add),
                                     [PS[by], gate, yacc], [yacc])
            K.barrier()
            A.release(moe_w_mark)
            xr2 = [A.alloc("xr2_%d" % i, [128, D], F32) for i in range(4)]
            tmp2 = [A.alloc("tmp2_%d" % i, [128, D], F32) for i in range(2)]
            ot = [A.alloc("ot%d" % i, [128, D], F32) for i in range(4)]
            st2 = [A.alloc("st2_%d" % i, [128, 16], F32) for i in range(4)]

            def l2_a(tt):
                j = tt % 4
                K.dma("sp", [(xr2[j].ap, x1s_d[b, tt * 128:(tt + 1) * 128, :])], xr2[j],
                      reads=[x1s_res[b][tt]], writes=[xr2[j]])

            def l2_b(tt):
                j = tt % 4
                K.op("dve", lambda e: e.scalar_tensor_tensor(out=xr2[j].ap, in0=xr2[j].ap, scalar=ALPHA,
                                                             in1=yacc.ap[:, tt, :], op0=ALU.mult, op1=ALU.add),
                     [xr2[j], yacc], [xr2[j]])
                ln_stats(xr2[j], st2[j])

            def l2_c(tt):
                ln_rstd(st2[tt % 4])

            def l2_d(tt):
                j = tt % 4
                ln_norm(xr2[j], 2, ot[j], tmp2[tt % 2], st2[j])

            def l2_e(tt):
                j = tt % 4
                K.dma("sp", [(out_d[b, tt * 128:(tt + 1) * 128, :], ot[j].ap)], ot[j], reads=[ot[j]])

            stages = [l2_e, l2_d, l2_c, l2_b]
            nst = len(stages)
            for i in range(NT + nst):
                for k, fn_ in enumerate(stages):
                    tt_ = i - (nst - k)
                    if 0 <= tt_ < NT:
                        fn_(tt_)
                if i < NT:
                    l2_a(i)
          except _Stop:
            pass
        K.barrier()
        K.final_wait()
        nc._arena_where = A.where
    return nc


def _consts():
    ident = np.eye(128, dtype=np.float32)
    perm = np.zeros((128, 128), np.float32)
    for m in range(64):
        perm[m + 64, m] = 1.0
        perm[m, m + 64] = 1.0
    ones = np.ones((128, 128), np.float32)
    kk = np.arange(128)[:, None]
    qq = np.arange(128)[None, :]
    m_cur = np.where(qq >= kk, 0.0, NEG).astype(np.float32)
    m_prev = np.where(qq <= kk, 0.0, NEG).astype(np.float32)
    m_all = np.full((128, 128), NEG, np.float32)
    ustrict = (kk < qq).astype(np.float32)
    cbf = np.stack([ident, perm, ones, m_cur, m_prev, m_all, ustrict, m_cur, m_all], axis=1).astype(ml_dtypes.bfloat16)
    half = 64
    inv = (np.float32(10000.0) ** (-np.arange(half, dtype=np.float32) / np.float32(half))).astype(np.float32)
    cf = np.zeros((128, 4), np.float32)
    cf[:, 0] = np.concatenate([inv, inv])
    cf[:, 1] = np.concatenate([-np.ones(64), np.ones(64)])
    cf[:, 2] = np.arange(128)
    onesf = np.full((128, 128), 1.0 / 512.0, np.float32)
    return cbf, cf, onesf


def _prep_shared(w_in, b_in, conv_w, conv_b, conv_ln_g, conv_ln_b, w_mem_kv, w_attn_o, w_conv_o, w_mem_o,
                 w_out, ln1_g, ln1_b, w_group_router, b_group_router, w_expert_router, b_expert_router,
                 w_exp_gate, w_exp_up, w_exp_down, ln2_g, ln2_b):
    f = lambda a: np.ascontiguousarray(np.asarray(a, dtype=np.float32))
    cbf, cf, onesf = _consts()
    sh = {}
    wi = f(w_in)[0]
    sh["w_in_r"] = f(wi.reshape(8, 128, 72, 128).transpose(2, 1, 0, 3))
    sh["b_inT"] = f(f(b_in)[0].reshape(72, 128).T)
    sh["b_v"] = f(f(b_in)[0][3072:4608][None, :])
    sh["conv_wT"] = f(f(conv_w)[0].reshape(31, 4, 128).transpose(2, 1, 0))
    sh["conv_p"] = f(np.stack([f(conv_b)[0].reshape(4, 128).T, f(conv_ln_g)[0].reshape(4, 128).T,
                               f(conv_ln_b)[0].reshape(4, 128).T], axis=1))
    sh["w_kv_r"] = f(f(w_mem_kv)[0].reshape(8, 128, 1024).transpose(1, 0, 2))
    sh["w_ao_r"] = f(f(w_attn_o)[0].reshape(4, 128, 1024).transpose(1, 0, 2))
    sh["w_co_r"] = f(f(w_conv_o)[0].reshape(4, 128, 1024).transpose(1, 0, 2))
    sh["w_mo_r"] = f(f(w_mem_o)[0].reshape(4, 128, 1024).transpose(1, 0, 2))
    sh["w_out_r"] = f(f(w_out)[0].reshape(8, 128, 1024).transpose(1, 0, 2))
    sh["ln_p"] = f(np.stack([f(ln1_g)[0], f(ln1_b)[0], f(ln2_g)[0], f(ln2_b)[0]], axis=0))
    wr = np.concatenate([f(w_group_router)[0]] + [f(w_expert_router)[0][g] for g in range(4)], axis=1)
    sh["w_r_r"] = f(wr.reshape(8, 128, 20).transpose(1, 0, 2))
    sh["b_r"] = f(np.concatenate([f(b_group_router)[0], f(b_expert_router)[0].reshape(16)])[None, :])
    sh["w_g_r"] = f(f(w_exp_gate)[0].reshape(16, 8, 128, 512).transpose(0, 2, 1, 3))
    sh["w_u_r"] = f(f(w_exp_up)[0].reshape(16, 8, 128, 512).transpose(0, 2, 1, 3))
    sh["w_d_r"] = f(f(w_exp_down)[0].reshape(16, 4, 128, 1024).transpose(0, 2, 1, 3))
    sh["c_bf"] = cbf
    sh["c_f32"] = cf
    sh["c_onesf"] = onesf
    sh["c_iota"] = np.ascontiguousarray(np.tile(np.arange(NSLOT, dtype=np.float32)[None, :], (128, 1)))
    return sh


def kernel(x, mem, positions, **w):
    x = np.asarray(x, dtype=np.float32)
    mem = np.asarray(mem, dtype=np.float32)
    positions = np.asarray(positions, dtype=np.int32)
    B = x.shape[0]
    nb = B // NCORES
    sh = _prep_shared(**w)
    nc = build_program(nb)
    in_maps = []
    for c in range(NCORES):
        m = dict(sh)
        m["x"] = np.ascontiguousarray(x[c * nb:(c + 1) * nb])
        m["mem"] = np.ascontiguousarray(mem[c * nb:(c + 1) * nb])
        m["pos"] = np.ascontiguousarray(positions[c * nb:(c + 1) * nb])
        in_maps.append(m)
    res = run_bass_kernel_spmd(nc, in_maps, core_ids=list(range(NCORES)))
    return np.concatenate([np.asarray(r["out"], dtype=np.float32) for r in res.results], axis=0)
```

```python
import math
from contextlib import ExitStack

import numpy as np
import ml_dtypes

import concourse.bass as bass
import concourse.mybir as mybir
from concourse.bass_utils import run_bass_kernel_spmd

F32 = mybir.dt.float32
BF16 = mybir.dt.bfloat16
I32 = mybir.dt.int32
ALU = mybir.AluOpType
AF = mybir.ActivationFunctionType
AX = mybir.AxisListType

NCORES = 8
S = 2048
D = 1024
MEM = 256
NT = S // 128
NTB = S // 512
DIL = (1, 4, 16)
ALPHA = 2.0 ** 0.25
EPS = 1e-5
SCALE = 128.0 ** -0.5
NEG = -30000.0
TWO_PI = 2.0 * math.pi
C1 = 6.28125
C2 = TWO_PI - C1
ARENA_BYTES = 204 * 1024
NSLOT = 48
SPARSE = True
SKIP_UNUSED = False


def _dsize(dt):
    return 2 if dt == BF16 else 4


class Res:
    __slots__ = ("name", "w", "r", "dsem", "excl")

    def __init__(self, name, excl=False):
        self.name = name
        self.excl = excl
        self.w = None
        self.r = {}
        self.dsem = None


class T:
    __slots__ = ("ap", "res")

    def __init__(self, ap, res):
        self.ap = ap
        self.res = res


class Eng:
    def __init__(self, name, h, sem):
        self.name = name
        self.h = h
        self.sem = sem
        self.cnt = 0
        self.pending = False
        self.waited = {}


class KB:
    def __init__(self, nc, es):
        self.nc = nc
        self.es = es
        self.E = {}
        for n, h in (("pe", nc.tensor), ("act", nc.scalar), ("dve", nc.vector),
                     ("pool", nc.gpsimd), ("sp", nc.sync)):
            sem = es.enter_context(nc.semaphore("sem_" + n))
            self.E[n] = Eng(n, h, sem)
        self.semown = {id(e.sem): e for e in self.E.values()}
        self.dsems = []
        self.dsem_by_name = {}
        self.nsem = 0

    def _wait(self, eng, ev):
        sem, val = ev
        k = id(sem)
        if eng.waited.get(k, 0) >= val:
            return
        own = self.semown.get(k)
        if own is not None:
            assert val <= own.cnt, "wait on a signal that was never issued (%s)" % own.name
        eng.h.wait_ge(sem, val)
        eng.waited[k] = val

    def _deps(self, reads, writes):
        evs = []
        for r in reads:
            if r.w is not None:
                evs.append(r.w)
        for w in writes:
            if w.w is not None:
                evs.append(w.w)
            evs.extend(w.r.values())
        return evs

    def op(self, en, fn, reads=(), writes=(), signal=True):
        eng = self.E[en]
        reads = [t.res if isinstance(t, T) else t for t in reads]
        writes = [t.res if isinstance(t, T) else t for t in writes]
        writes = writes + [r for r in reads if r.excl and r not in writes]
        for ev in self._deps(reads, writes):
            if ev[0] is eng.sem and en == "pe":
                continue
            self._wait(eng, ev)
        ins = fn(eng.h)
        if signal:
            eng.cnt += 1
            ins.then_inc(eng.sem, 1)
            ev = (eng.sem, eng.cnt)
            eng.pending = False
        else:
            ev = (eng.sem, eng.cnt + 1)
            eng.pending = True
        k = id(eng.sem)
        for r in reads:
            r.r[k] = ev
        for w in writes:
            w.w = ev
            w.r = {}

    def _dsem(self, sres, en):
        key = (sres.name, en)
        ds = self.dsem_by_name.get(key)
        if ds is None:
            self.nsem += 1
            ds = [self.es.enter_context(self.nc.semaphore("dsem%d" % self.nsem)), 0]
            self.dsems.append(ds)
            self.dsem_by_name[key] = ds
        return ds

    def dma(self, en, pairs, sres, reads=(), writes=()):
        eng = self.E[en]
        sres = sres.res if isinstance(sres, T) else sres
        reads = [t.res if isinstance(t, T) else t for t in reads]
        writes = [t.res if isinstance(t, T) else t for t in writes]
        for ev in self._deps(reads, writes):
            self._wait(eng, ev)
        ds = self._dsem(sres, en)
        for o, i in pairs:
            eng.h.dma_start(out=o, in_=i).then_inc(ds[0], 16)
            ds[1] += 16
        ev = (ds[0], ds[1])
        k = id(ev[0])
        for r in reads:
            r.r[k] = ev
        for w in writes:
            w.w = ev
            w.r = {}

    def idma(self, out, out_idx, in_, in_idx, sres, bound, reads=(), writes=()):
        eng = self.E["pool"]
        sres = sres.res if isinstance(sres, T) else sres
        reads = [t.res if isinstance(t, T) else t for t in reads]
        writes = [t.res if isinstance(t, T) else t for t in writes]
        for ev in self._deps(reads, writes):
            self._wait(eng, ev)
        ds = self._dsem(sres, "pool_ind")
        oo = bass.IndirectOffsetOnAxis(ap=out_idx, axis=0) if out_idx is not None else None
        io = bass.IndirectOffsetOnAxis(ap=in_idx, axis=0) if in_idx is not None else None
        if bound is None:
            eng.h.indirect_dma_start(out=out, out_offset=oo, in_=in_, in_offset=io).then_inc(ds[0], 16)
        else:
            eng.h.indirect_dma_start(out=out, out_offset=oo, in_=in_, in_offset=io, bounds_check=bound,
                                     oob_is_err=False).then_inc(ds[0], 16)
        ds[1] += 16
        ev = (ds[0], ds[1])
        k = id(ev[0])
        for r in reads:
            r.r[k] = ev
        for w in writes:
            w.w = ev
            w.r = {}

    def barrier(self, engines=None):
        for e in self.E.values():
            assert not e.pending
        for e in self.E.values():
            for x in self.E.values():
                if x is not e and x.cnt > 0:
                    self._wait(e, (x.sem, x.cnt))
            for ds in self.dsems:
                if ds[1] > 0:
                    self._wait(e, (ds[0], ds[1]))

    def final_wait(self):
        e = self.E["sp"]
        for ds in self.dsems:
            if ds[1] > 0:
                self._wait(e, (ds[0], ds[1]))


class Arena:
    def __init__(self, ap):
        self.ap = ap
        self.off = 0
        self.peak = 0
        self.top = ARENA_BYTES
        self.where = {}

    def alloc(self, name, shape, dt, top=False):
        n = 1
        for s in shape[1:]:
            n *= s
        nb = n * _dsize(dt)
        nb = (nb + 63) // 64 * 64
        if top:
            self.top -= nb
            start = self.top
        else:
            start = self.off
            self.off += nb
        assert self.off <= self.top, "arena overflow at %s: %d/%d" % (name, self.off, self.top)
        v = self.ap[:, start // 4:(start + nb) // 4]
        if dt != F32:
            v = v.bitcast(dt)
        v = v[:, 0:n]
        if len(shape) == 3:
            v = v.rearrange("p (a b) -> p a b", a=shape[1])
        elif len(shape) == 4:
            v = v.rearrange("p (a b c) -> p a b c", a=shape[1], b=shape[2])
        if shape[0] != 128:
            v = v[0:shape[0]]
        self.peak = max(self.peak, self.off + ARENA_BYTES - self.top)
        self.where[name] = (start, n, dt, tuple(shape))
        return T(v, Res(name))

    def mark(self):
        return self.off

    def release(self, m):
        self.off = m


class _Stop(Exception):
    pass


def build_program(nb, stage=None):
    nc = bass.Bass("TRN2", target_bir_lowering=False)

    def din(name, shape, dt=F32):
        return nc.dram_tensor(name, list(shape), dt, kind="ExternalInput").ap()

    x_d = din("x", [nb, S, D])
    mem_d = din("mem", [nb, MEM, D])
    pos_d = din("pos", [nb, S], I32)
    win_d = din("w_in_r", [72, 128, 8, 128])
    binT_d = din("b_inT", [128, 72])
    bv_d = din("b_v", [1, 1536])
    convw_d = din("conv_wT", [128, 4, 31])
    convp_d = din("conv_p", [128, 3, 4])
    wkv_d = din("w_kv_r", [128, 8, 1024])
    wao_d = din("w_ao_r", [128, 4, 1024])
    wco_d = din("w_co_r", [128, 4, 1024])
    wmo_d = din("w_mo_r", [128, 4, 1024])
    wout_d = din("w_out_r", [128, 8, 1024])
    lnp_d = din("ln_p", [4, 1024])
    wr_d = din("w_r_r", [128, 8, 20])
    br_d = din("b_r", [1, 20])
    wg_d = din("w_g_r", [16, 128, 8, 512])
    wu_d = din("w_u_r", [16, 128, 8, 512])
    wd_d = din("w_d_r", [16, 128, 4, 1024])
    cbf_d = din("c_bf", [128, 9, 128], BF16)
    iota_d = din("c_iota", [128, NSLOT])
    cf_d = din("c_f32", [128, 4])
    onesf_d = din("c_onesf", [128, 128])
    out_d = nc.dram_tensor("out", [nb, S, D], F32, kind="ExternalOutput").ap()
    x1s_d = nc.dram_tensor("x1_scratch", [nb, S, D], F32, kind="Internal").ap()
    x1s_res = [[Res("x1s%d_%d" % (b, t)) for t in range(NT)] for b in range(nb)]
    x1h_d = nc.dram_tensor("x1h_scratch", [nb, S, D], BF16, kind="Internal").ap()
    x1h_res = [[Res("x1h%d_%d" % (b, t)) for t in range(NT)] for b in range(nb)]
    xs_d = nc.dram_tensor("xs_scratch", [NSLOT * 128, D], BF16, kind="Internal").ap()
    ys_d = nc.dram_tensor("ys_scratch", [NSLOT * 128, D], F32, kind="Internal").ap()
    wgb_d = nc.dram_tensor("wgb_scratch", [16 * 128, 4096], BF16, kind="Internal").ap()
    wub_d = nc.dram_tensor("wub_scratch", [16 * 128, 4096], BF16, kind="Internal").ap()
    wdb_d = nc.dram_tensor("wdb_scratch", [16 * 128, 4096], BF16, kind="Internal").ap()

    with ExitStack() as es:
        K = KB(nc, es)
        arena_t = es.enter_context(nc.sbuf_tensor("arena", [128, ARENA_BYTES // 4], F32))
        A = Arena(arena_t[:])
        PS = []
        for i in range(8):
            pt = es.enter_context(nc.psum_tensor("ps%d" % i, [128, 512], F32))
            PS.append(T(pt[:], Res("ps%d" % i, excl=True)))

        def psb(i):
            return PS[i].ap.bitcast(BF16)

        cbf = A.alloc("cbf", [128, 9, 128], BF16)
        iota = A.alloc("iota", [128, NSLOT], F32)
        cf = A.alloc("cf", [128, 4], F32)
        onesf = A.alloc("onesf", [128, 128], F32)
        binT = A.alloc("binT", [128, 72], F32)
        convw = A.alloc("convw", [128, 4, 31], F32)
        convp = A.alloc("convp", [128, 3, 4], F32)
        lnp = A.alloc("lnp", [128, 4, 1024], F32)
        wr_hi = A.alloc("wr_hi", [128, 8, 20], BF16)
        wr_lo = A.alloc("wr_lo", [128, 8, 20], BF16)
        wr_f = A.alloc("wr_f", [128, 8, 20], F32)
        wr_t = A.alloc("wr_t", [128, 8, 20], F32)
        brb = A.alloc("brb", [128, 20], F32)
        K.dma("sp", [(cbf.ap, cbf_d)], cbf, writes=[cbf])
        K.dma("sp", [(cf.ap, cf_d)], cf, writes=[cf])
        K.dma("sp", [(iota.ap, iota_d)], iota, writes=[iota])
        K.dma("sp", [(onesf.ap, onesf_d)], onesf, writes=[onesf])
        K.dma("sp", [(binT.ap, binT_d)], binT, writes=[binT])
        K.dma("sp", [(convw.ap, convw_d)], convw, writes=[convw])
        K.dma("sp", [(convp.ap, convp_d)], convp, writes=[convp])
        K.dma("sp", [(lnp.ap[:, i, :], lnp_d[i:i + 1, :].broadcast_to([128, 1024])) for i in range(4)],
              lnp, writes=[lnp])
        K.dma("sp", [(wr_f.ap, wr_d)], wr_f, writes=[wr_f])
        K.dma("sp", [(brb.ap, br_d.broadcast_to([128, 20]))], brb, writes=[brb])
        K.op("dve", lambda e: e.tensor_copy(out=wr_hi.ap, in_=wr_f.ap), [wr_f], [wr_hi])
        K.op("dve", lambda e: e.tensor_tensor(out=wr_t.ap, in0=wr_f.ap, in1=wr_hi.ap, op=ALU.subtract),
             [wr_f, wr_hi], [wr_t])
        K.op("dve", lambda e: e.tensor_copy(out=wr_lo.ap, in_=wr_t.ap), [wr_t], [wr_lo])
        ident = cbf.ap[:, 0, :]
        perm = cbf.ap[:, 1, :]
        onesb = cbf.ap[:, 2, :]
        m_cur = cbf.ap[:, 3, :]
        m_prev = cbf.ap[:, 4, :]
        m_all = cbf.ap[:, 5, :]
        invf = cf.ap[:, 0:1]
        sgn = cf.ap[:, 1:2]
        pidx = cf.ap[:, 2:3]
        ustrict = cbf.ap[:, 6, :]
        m_cp = cbf.ap[:, 3:5, :].rearrange("p a b -> p (a b)")
        m_ca = cbf.ap[:, 7:9, :].rearrange("p a b -> p (a b)")

        base_mark = A.mark()
        wbound = None
        if SKIP_UNUSED:
            bc_reg = nc.gpsimd.alloc_register("wbound")
            nc.gpsimd.reg_mov(bc_reg, 16 * 128 - 1)
            wbound = nc.gpsimd.snap(bc_reg)
        precast_q = []
        wres = Res("wscratch")
        if SPARSE:
            for ex in range(16):
                for src_, dst_ in ((wg_d, wgb_d), (wu_d, wub_d), (wd_d, wdb_d)):
                    precast_q.append((dst_[ex * 128:(ex + 1) * 128, :], src_[ex].rearrange("p a b -> p (a b)")))
            zt = A.alloc("zt", [128, D], BF16)
            K.op("dve", lambda e: e.memset(zt.ap, 0.0), [], [zt])
            base_mark = A.mark()

        def issue_precast(n):
            for _ in range(n):
                if precast_q:
                    o_, i_ = precast_q.pop(0)
                    K.dma("pool", [(o_, i_)], wres)

        def mm(out, lhsT, rhs, start, stop, reads, bank, signal, skip=False):
            if skip:
                K.op("pe", lambda e: e.matmul(out, lhsT, rhs, start=start, stop=stop, skip_group_check=True),
                     reads, [bank], signal=signal)
            else:
                K.op("pe", lambda e: e.matmul(out, lhsT, rhs, start=start, stop=stop),
                     reads, [bank], signal=signal)

        def transpose_to(bank_i, col0, in_ap, reads, signal):
            o = psb(bank_i)[:, col0:col0 + 128]
            K.op("pe", lambda e: e.transpose(o, in_ap, ident), list(reads) + [cbf], [PS[bank_i]],
                 signal=signal)

        evac_flip = [0]

        def copy_any(out, in_, reads, writes):
            evac_flip[0] ^= 1
            if evac_flip[0]:
                K.op("act", lambda e: e.copy(out=out, in_=in_), reads, writes)
            else:
                K.op("dve", lambda e: e.tensor_copy(out=out, in_=in_), reads, writes)

        def ln_stats(v, st):
            stats = st.ap[:, 0:12]
            mv = st.ap[:, 12:14]
            K.op("dve", lambda e: e.bn_stats(out=stats[:, 0:6], in_=v.ap[:, 0:512]), [v], [st])
            K.op("dve", lambda e: e.bn_stats(out=stats[:, 6:12], in_=v.ap[:, 512:1024]), [v], [st])
            K.op("dve", lambda e: e.bn_aggr(out=mv, in_=stats), [st], [st])

        def ln_rstd(st):
            mv = st.ap[:, 12:14]
            K.op("act", lambda e: e.activation(out=st.ap[:, 14:15], in_=mv[:, 1:2], func=AF.Sqrt,
                                               bias=EPS, scale=1.0), [st], [st])
            K.op("dve", lambda e: e.reciprocal(out=st.ap[:, 14:15], in_=st.ap[:, 14:15]), [st], [st])
            K.op("dve", lambda e: e.scalar_tensor_tensor(out=st.ap[:, 15:16], in0=mv[:, 0:1], scalar=-1.0,
                                                         in1=st.ap[:, 14:15], op0=ALU.mult, op1=ALU.mult),
                 [st], [st])

        def ln_norm(v, gi, out_t, tmp, st):
            K.op("act", lambda e: e.activation(out=tmp.ap, in_=v.ap, func=AF.Identity,
                                               bias=st.ap[:, 15:16], scale=st.ap[:, 14:15]), [v, st], [tmp])
            K.op("dve", lambda e: e.tensor_tensor(out=tmp.ap, in0=tmp.ap, in1=lnp.ap[:, gi, :], op=ALU.mult),
                 [tmp, lnp], [tmp])
            K.op("pool", lambda e: e.tensor_tensor(out=out_t.ap, in0=tmp.ap, in1=lnp.ap[:, gi + 1, :],
                                                   op=ALU.add), [tmp, lnp], [out_t])

        def sparse_moe(b, oha, ohb, wts, stage=stage):
            NE = 16
            Mb = A.alloc("Mb", [128, NT * NE], BF16)
            tmpf = A.alloc("tmpf", [128, NT, NE], F32)
            tot = A.alloc("tot", [128, NT, NE], F32)
            cum = A.alloc("cum", [128, NT + 1, NE], F32)
            posf = A.alloc("posf", [128, NT, NE], F32)
            sml = A.alloc("sml", [128, 8, NE], F32)
            smi = A.alloc("smi", [128, NE], I32)
            posa = A.alloc("posa", [128, 2, NT], F32)
            posi = A.alloc("posi", [128, 2, NT], I32)
            cmp3 = A.alloc("cmp3", [128, NSLOT, NE], F32)
            ejf = A.alloc("ejf", [128, NSLOT], F32)
            widx = A.alloc("widx", [128, NSLOT], I32)
            top_save = A.top
            xall = A.alloc("xall", [128, NT, D], BF16, top=True)
            K.dma("sp", [(xall.ap[:, tt, :], x1h_d[b, tt * 128:(tt + 1) * 128, :]) for tt in range(NT)], xall,
                  reads=[x1h_res[b][tt] for tt in range(NT)], writes=[xall])
            dvo = lambda fn_, r_, w_: K.op("dve", fn_, r_, w_)
            dvo(lambda e: e.tensor_tensor(out=tmpf.ap, in0=oha.ap, in1=ohb.ap, op=ALU.add), [oha, ohb], [tmpf])
            dvo(lambda e: e.tensor_copy(out=Mb.ap, in_=tmpf.ap.rearrange("p t e -> p (t e)")), [tmpf], [Mb])
            mm(PS[0].ap[:, 0:NT * NE], ustrict, Mb.ap, True, True, [cbf, Mb], PS[0], True)
            mm(PS[1].ap[:, 0:NT * NE], onesb, Mb.ap, True, True, [cbf, Mb], PS[1], True)
            dvo(lambda e: e.tensor_copy(out=tot.ap.rearrange("p t e -> p (t e)"), in_=PS[1].ap[:, 0:NT * NE]),
                [PS[1]], [tot])
            dvo(lambda e: e.memset(cum.ap[:, 0, :], 0.0), [], [cum])
            for tt in range(NT):
                dvo(lambda e: e.tensor_tensor(out=cum.ap[:, tt + 1, :], in0=cum.ap[:, tt, :], in1=tot.ap[:, tt, :],
                                              op=ALU.add), [cum, tot], [cum])
            cnt_ = cum.ap[:, NT, :]
            ntf, endt, base_ = sml.ap[:, 0, :], sml.ap[:, 1, :], sml.ap[:, 2, :]
            dvo(lambda e: e.tensor_scalar(out=sml.ap[:, 3, :], in0=cnt_, scalar1=1.0 / 128.0,
                                          scalar2=127.0 / 128.0 - 0.5 + 1.0 / 256.0, op0=ALU.mult, op1=ALU.add),
                [cum], [sml])
            dvo(lambda e: e.tensor_copy(out=smi.ap, in_=sml.ap[:, 3, :]), [sml], [smi])
            dvo(lambda e: e.tensor_copy(out=ntf, in_=smi.ap), [smi], [sml])
            dvo(lambda e: e.tensor_copy(out=endt[:, 0:1], in_=ntf[:, 0:1]), [sml], [sml])
            for ex in range(1, NE):
                dvo(lambda e: e.tensor_tensor(out=endt[:, ex:ex + 1], in0=endt[:, ex - 1:ex], in1=ntf[:, ex:ex + 1],
                                              op=ALU.add), [sml], [sml])
            dvo(lambda e: e.tensor_tensor(out=base_, in0=endt, in1=ntf, op=ALU.subtract), [sml], [sml])
            dvo(lambda e: e.tensor_scalar(out=base_, in0=base_, scalar1=128.0, scalar2=None, op0=ALU.mult),
                [sml], [sml])
            dvo(lambda e: e.tensor_tensor(out=posf.ap.rearrange("p t e -> p (t e)"), in0=PS[0].ap[:, 0:NT * NE],
                                          in1=cum.ap[:, 0:NT, :].rearrange("p t e -> p (t e)"), op=ALU.add),
                [PS[0], cum], [posf])
            dvo(lambda e: e.tensor_tensor(out=posf.ap, in0=posf.ap,
                                          in1=base_.unsqueeze(1).broadcast_to([128, NT, NE]), op=ALU.add),
                [posf, sml], [posf])
            for si, oh_ in enumerate((oha, ohb)):
                dvo(lambda e: e.tensor_tensor(out=tmpf.ap, in0=posf.ap, in1=oh_.ap, op=ALU.mult),
                    [posf, oh_], [tmpf])
                dvo(lambda e: e.tensor_reduce(out=posa.ap[:, si, :], in_=tmpf.ap, axis=AX.X, op=ALU.add),
                    [tmpf], [posa])
            dvo(lambda e: e.tensor_copy(out=posi.ap, in_=posa.ap), [posa], [posi])
            dvo(lambda e: e.tensor_tensor(out=cmp3.ap, in0=iota.ap.unsqueeze(2).broadcast_to([128, NSLOT, NE]),
                                          in1=endt.unsqueeze(1).broadcast_to([128, NSLOT, NE]), op=ALU.is_ge),
                [iota, sml], [cmp3])
            dvo(lambda e: e.tensor_reduce(out=ejf.ap, in_=cmp3.ap, axis=AX.X, op=ALU.add), [cmp3], [ejf])
            if SKIP_UNUSED:
                dvo(lambda e: e.tensor_scalar(out=ejf.ap, in0=ejf.ap, scalar1=128.0, scalar2=None,
                                              op0=ALU.mult), [ejf], [ejf])
            else:
                dvo(lambda e: e.tensor_scalar(out=ejf.ap, in0=ejf.ap, scalar1=float(NE - 1), scalar2=128.0,
                                              op0=ALU.min, op1=ALU.mult), [ejf], [ejf])
            dvo(lambda e: e.tensor_scalar(out=ejf.ap, in0=ejf.ap, scalar1=pidx, scalar2=None, op0=ALU.add),
                [ejf, cf], [ejf])
            dvo(lambda e: e.tensor_copy(out=widx.ap, in_=ejf.ap), [ejf], [widx])
            if stage == "S1":
                raise _Stop()
            xs_res = Res("xs")
            for tt in range(NT):
                for si in range(2):
                    K.idma(xs_d, posi.ap[:, si, tt:tt + 1], xall.ap[:, tt, :], None, xall, None,
                           reads=[xall, posi])
            K.barrier()
            A.top = top_save
            sp_mark = A.mark()
            if stage == "S2":
                raise _Stop()
            NB_ = 3
            NBD = 5
            wgs = [A.alloc("wgs%d" % i, [128, 8, 512], BF16) for i in range(NB_)]
            wus = [A.alloc("wus%d" % i, [128, 8, 512], BF16) for i in range(NB_)]
            wds = [A.alloc("wds%d" % i, [128, 4, 1024], BF16) for i in range(NBD)]
            xst = [A.alloc("xst%d" % i, [128, D], BF16) for i in range(NB_)]
            xsT = [A.alloc("xsT%d" % i, [128, 8, 128], BF16) for i in range(NB_)]
            sgs = [A.alloc("sgs%d" % i, [128, 512], F32) for i in range(NB_)]
            hds = [A.alloc("hds%d" % i, [128, 4, 128], BF16) for i in range(NB_)]
            ysb = [A.alloc("ysb%d" % i, [128, D], F32) for i in range(NB_)]
            ys_res = Res("ys")

            def m_load(j):
                k = j % NB_
                K.dma("sp", [(xst[k].ap, xs_d[j * 128:(j + 1) * 128, :])], xst[k], writes=[xst[k]])
                for t_, d_, a_ in zip((wgs[k], wus[k], wds[j % NBD]), (wgb_d, wub_d, wdb_d), (8, 8, 4)):
                    K.idma(t_.ap.rearrange("p a b -> p (a b)"), None, d_, widx.ap[:, j:j + 1], t_,
                           wbound, reads=[widx], writes=[t_])

            def m_tr(j):
                k = j % NB_
                bk = j % 2
                for kc in range(8):
                    transpose_to(bk, kc * 128, xst[k].ap[:, kc * 128:(kc + 1) * 128], [xst[k]], kc == 7)

            def m_cp(j):
                k = j % NB_
                copy_any(xsT[k].ap, psb(j % 2).rearrange("p (a b) -> p a b", a=8), [PS[j % 2]], [xsT[k]])

            def m_gu(j):
                k = j % NB_
                wg_, wu_ = wgs[k], wus[k]
                for w_, bk in ((wg_, 2 + j % 2), (wu_, 4 + j % 2)):
                    for fc in range(4):
                        for kc in range(8):
                            mm(PS[bk].ap[:, fc * 128:(fc + 1) * 128], w_.ap[:, kc, fc * 128:(fc + 1) * 128],
                               xsT[k].ap[:, kc, :], kc == 0, kc == 7, [w_, xsT[k]], PS[bk],
                               fc == 3 and kc == 7)

            def m_act(j):
                k = j % NB_
                K.op("act", lambda e: e.activation(out=sgs[k].ap, in_=PS[2 + j % 2].ap, func=AF.Silu),
                     [PS[2 + j % 2]], [sgs[k]])
                K.op("dve", lambda e: e.tensor_tensor(out=hds[k].ap.rearrange("p a b -> p (a b)"),
                                                      in0=PS[4 + j % 2].ap, in1=sgs[k].ap, op=ALU.mult),
                     [PS[4 + j % 2], sgs[k]], [hds[k]])

            def m_dn(j):
                k = j % NB_
                wd_ = wds[j % NBD]
                for dh in range(2):
                    for fc in range(4):
                        mm(PS[6 + dh].ap, hds[k].ap[:, fc, :], wd_.ap[:, fc, dh * 512:(dh + 1) * 512],
                           fc == 0, fc == 3, [hds[k], wd_], PS[6 + dh], fc == 3)

            def m_ev(j):
                k = j % NB_
                K.op("act", lambda e: e.copy(out=ysb[k].ap[:, 0:512], in_=PS[6].ap), [PS[6]], [ysb[k]])
                K.op("dve", lambda e: e.tensor_copy(out=ysb[k].ap[:, 512:1024], in_=PS[7].ap), [PS[7]], [ysb[k]])
                K.dma("sp", [(ys_d[j * 128:(j + 1) * 128, :], ysb[k].ap)], ysb[k], reads=[ysb[k]],
                      writes=[ys_res])

            m_load(0)
            stages = [(m_ev, 4), (m_dn, 3), (m_act, 2), (m_cp, 1), (m_gu, 1), (m_tr, 0)]
            if stage == "S3a":
                stages = []
            elif stage == "S3b":
                stages = stages[3:]
            elif stage == "S3c":
                stages = stages[2:]
            nsl = 6 if stage in ("S3a", "S3b", "S3c") else NSLOT
            for i in range(nsl + 4):
                for fn_, d_ in stages:
                    j_ = i - d_
                    if 0 <= j_ < nsl:
                        fn_(j_)
                if i + 1 < nsl:
                    m_load(i + 1)
            if stage in ("S3a", "S3b", "S3c"):
                K.barrier()
                raise _Stop()
            K.barrier()
            if stage == "S3":
                raise _Stop()
            A.release(sp_mark)
            xr2 = [A.alloc("xr2_%d" % i, [128, D], F32) for i in range(5)]
            yga = [A.alloc("yga%d" % i, [128, D], F32) for i in range(3)]
            ygb = [A.alloc("ygb%d" % i, [128, D], F32) for i in range(3)]
            tmp2 = [A.alloc("tmp2_%d" % i, [128, D], F32) for i in range(2)]
            ot = [A.alloc("ot%d" % i, [128, D], F32) for i in range(3)]
            st2 = [A.alloc("st2_%d" % i, [128, 16], F32) for i in range(4)]

            def l_a(tt):
                j = tt % 3
                K.dma("sp", [(xr2[tt % 5].ap, x1s_d[b, tt * 128:(tt + 1) * 128, :])], xr2[tt % 5],
                      reads=[x1s_res[b][tt]], writes=[xr2[tt % 5]])
                K.idma(yga[j].ap, None, ys_d, posi.ap[:, 0, tt:tt + 1], yga[j], None,
                       reads=[posi, ys_res], writes=[yga[j]])
                K.idma(ygb[j].ap, None, ys_d, posi.ap[:, 1, tt:tt + 1], ygb[j], None,
                       reads=[posi, ys_res], writes=[ygb[j]])

            def l_b(tt):
                j = tt % 3
                K.op("dve", lambda e: e.scalar_tensor_tensor(out=xr2[tt % 5].ap, in0=yga[j].ap,
                                                             scalar=wts.ap[:, 0, tt:tt + 1], in1=xr2[tt % 5].ap,
                                                             op0=ALU.mult, op1=ALU.add), [yga[j], wts, xr2[tt % 5]],
                     [xr2[tt % 5]])
                K.op("dve", lambda e: e.scalar_tensor_tensor(out=xr2[tt % 5].ap, in0=ygb[j].ap,
                                                             scalar=wts.ap[:, 1, tt:tt + 1], in1=xr2[tt % 5].ap,
                                                             op0=ALU.mult, op1=ALU.add), [ygb[j], wts, xr2[tt % 5]],
                     [xr2[tt % 5]])
                ln_stats(xr2[tt % 5], st2[tt % 4])

            def l_c(tt):
                ln_rstd(st2[tt % 4])

            def l_d(tt):
                j = tt % 3
                ln_norm(xr2[tt % 5], 2, ot[j], tmp2[tt % 2], st2[tt % 4])
                K.dma("sp", [(out_d[b, tt * 128:(tt + 1) * 128, :], ot[j].ap)], ot[j], reads=[ot[j]])

            stages = [(l_a, -1), (l_d, 3), (l_c, 2), (l_b, 1)]
            l_a(0)
            for i in range(NT + 3):
                for fn_, d_ in stages:
                    t2_ = i - d_
                    if 0 <= t2_ < NT and not (fn_ is l_a and t2_ == 0):
                        fn_(t2_)

        for b in range(nb):
          try:
            K.barrier()
            A.release(base_mark)
            A.top = ARENA_BYTES
            xT = A.alloc("xT", [128, 8, S], BF16)
            wring = [A.alloc("wring%d" % i, [128, 8, 128], BF16) for i in range(8)]
            oT = A.alloc("oT", [128, 4, S], BF16)
            cnT = A.alloc("cnT", [128, 4, S], BF16)
            omT = A.alloc("omT", [128, 4, S], BF16)
            o_mark = A.mark()
            worder = []
            for it_ in range(12):
                for kind_ in range(3):
                    worder.append(kind_ * 12 + (it_ % 3) * 4 + it_ // 3)
            for ch_ in range(4):
                worder += [36 + ch_, 40 + ch_]
            worder += [44, 45, 46, 47]
            for dc_ in range(8):
                worder += [48 + dc_, 56 + dc_, 64 + dc_]
            wstate = {"issued": 0, "used": 0}
            LOOK = 5

            def _issue_w():
                i = wstate["issued"]
                if i < len(worder):
                    w = wring[i % 8]
                    K.dma("pool", [(w.ap, win_d[worder[i]])], w, writes=[w])
                    wstate["issued"] += 1

            def nextW(c):
                i = wstate["used"]
                assert worder[i] == c, (i, worder[i], c)
                while wstate["issued"] < min(len(worder), i + 1 + LOOK):
                    _issue_w()
                wstate["used"] += 1
                return wring[i % 8]

            getW = nextW

            kmT = A.alloc("kmT", [128, 4, MEM], BF16)
            vm = A.alloc("vm", [128, 2, 512], BF16)
            pa_mark = A.mark()
            cosT = A.alloc("cosT", [128, S], F32)
            sinS = A.alloc("sinS", [128, S], F32)
            tb_mark = A.mark()
            posI = A.alloc("posI", [128, S], I32)
            ang = A.alloc("ang", [128, S], F32)
            tq = A.alloc("tq", [128, S], F32)
            ki = A.alloc("ki", [128, S], I32)
            K.dma("sp", [(posI.ap, pos_d[b:b + 1, :].broadcast_to([128, S]))], posI, writes=[posI])
            K.op("dve", lambda e: e.tensor_copy(out=ang.ap, in_=posI.ap), [posI], [ang])
            K.op("dve", lambda e: e.tensor_scalar(out=ang.ap, in0=ang.ap, scalar1=invf, scalar2=None,
                                                  op0=ALU.mult), [ang, cf], [ang])
            for tab, shift in ((cosT, math.pi / 2), (sinS, 0.0)):
                K.op("dve", lambda e: e.tensor_scalar(out=tq.ap, in0=ang.ap, scalar1=1.0 / TWO_PI,
                                                      scalar2=shift / TWO_PI, op0=ALU.mult, op1=ALU.add),
                     [ang], [tq])
                K.op("dve", lambda e: e.tensor_copy(out=ki.ap, in_=tq.ap), [tq], [ki])
                K.op("dve", lambda e: e.tensor_copy(out=tq.ap, in_=ki.ap), [ki], [tq])
                K.op("dve", lambda e: e.scalar_tensor_tensor(out=tab.ap, in0=tq.ap, scalar=-C1, in1=ang.ap,
                                                             op0=ALU.mult, op1=ALU.add), [tq, ang], [tab])
                K.op("dve", lambda e: e.scalar_tensor_tensor(out=tab.ap, in0=tq.ap, scalar=-C2, in1=tab.ap,
                                                             op0=ALU.mult, op1=ALU.add), [tq, tab], [tab])
                hi = math.pi - shift - 1e-5
                lo = -math.pi - shift + 1e-5
                K.op("dve", lambda e: e.tensor_scalar(out=tab.ap, in0=tab.ap, scalar1=hi, scalar2=lo,
                                                      op0=ALU.min, op1=ALU.max), [tab], [tab])
                if shift == 0.0:
                    K.op("act", lambda e: e.activation(out=tab.ap, in_=tab.ap, func=AF.Sin), [tab], [tab])
                else:
                    K.op("dve", lambda e: e.tensor_scalar(out=tab.ap, in0=tab.ap, scalar1=shift, scalar2=None,
                                                          op0=ALU.add), [tab], [tab])
                    K.op("act", lambda e: e.activation(out=tab.ap, in_=tab.ap, func=AF.Sin), [tab], [tab])
            K.op("dve", lambda e: e.tensor_scalar(out=sinS.ap, in0=sinS.ap, scalar1=sgn, scalar2=None,
                                                  op0=ALU.mult), [sinS, cf], [sinS])
            if SPARSE and b == 0:
                K.dma("sp", [(xs_d[j_ * 128:(j_ + 1) * 128, :], zt.ap) for j_ in range(NSLOT)], zt, reads=[zt])
            xb = [A.alloc("xb%d" % i, [128, D], BF16) for i in range(4)]
            memT = A.alloc("memT", [128, 8, MEM], BF16)
            wkv = A.alloc("wkv", [128, 8, 1024], BF16)
            for t0_ in range(3):
                K.dma("pool", [(xb[t0_].ap, x_d[b, t0_ * 128:(t0_ + 1) * 128, :])], xb[t0_], writes=[xb[t0_]])
            K.dma("pool", [(wkv.ap, wkv_d)], wkv, writes=[wkv])
            for tt in range(NT):
                t_ = xb[tt % 4]
                if tt + 3 < NT:
                    tn_ = xb[(tt + 3) % 4]
                    K.dma("pool", [(tn_.ap, x_d[b, (tt + 3) * 128:(tt + 4) * 128, :])], tn_, writes=[tn_])
                bk = tt % 2
                for kc in range(8):
                    transpose_to(bk, kc * 128, t_.ap[:, kc * 128:(kc + 1) * 128], [t_], kc == 7)
                copy_any(xT.ap[:, :, tt * 128:(tt + 1) * 128],
                         psb(bk).rearrange("p (a b) -> p a b", a=8), [PS[bk]], [xT])
            for tt in range(2):
                t_ = xb[tt % 2]
                K.dma("pool", [(t_.ap, mem_d[b, tt * 128:(tt + 1) * 128, :])], t_, writes=[t_])
                bk = 2 + tt
                for kc in range(8):
                    transpose_to(bk, kc * 128, t_.ap[:, kc * 128:(kc + 1) * 128], [t_], kc == 7)
                copy_any(memT.ap[:, :, tt * 128:(tt + 1) * 128],
                         psb(bk).rearrange("p (a b) -> p a b", a=8), [PS[bk]], [memT])
            for hm in range(4):
                bk = 4 + hm % 2
                for kc in range(8):
                    mm(PS[bk].ap[:, 0:MEM], wkv.ap[:, kc, hm * 128:(hm + 1) * 128], memT.ap[:, kc, :],
                       kc == 0, kc == 7, [wkv, memT], PS[bk], kc == 7)
                copy_any(kmT.ap[:, hm, :], PS[bk].ap[:, 0:MEM], [PS[bk]], [kmT])
            for mc in range(2):
                bk = 6 + mc
                for kc in range(8):
                    mm(PS[bk].ap, memT.ap[:, kc, mc * 128:(mc + 1) * 128], wkv.ap[:, kc, 512:1024],
                       kc == 0, kc == 7, [wkv, memT], PS[bk], kc == 7)
                copy_any(vm.ap[:, mc, :], PS[bk].ap, [PS[bk]], [vm])
            if stage == "B":
                raise _Stop()
            K.barrier()
            A.release(tb_mark)

            if stage == "T":
                raise _Stop()
            accO = A.alloc("accO", [128, S], F32)
            accD = A.alloc("accD", [128, S], F32)
            qkv = [[A.alloc("qkv%d_%d" % (i, j), [128, S], BF16) for j in range(3)] for i in range(2)]
            Vp = [A.alloc("Vp%d" % i, [128, 16, 128], BF16) for i in range(2)]
            qb_t = [A.alloc("qb%d" % i, [128, 512], BF16) for i in range(2)]
            t1_t = [A.alloc("t1_%d" % i, [128, 512], F32) for i in range(2)]
            t2_t = [A.alloc("t2_%d" % i, [128, 512], F32) for i in range(2)]
            pT_t = [A.alloc("pT%d" % i, [128, 512], BF16) for i in range(2)]
            cnt = {"proj": 0, "rot": 0, "st": 0, "od": 0, "gi": 0}

            def sub_view(ap2d, r, tb):
                if r == 1:
                    return ap2d[:, tb * 512:(tb + 1) * 512]
                L = S // r
                v = ap2d.rearrange("p (r u) -> p r u", r=r)
                return v[:, :, tb * (512 // r):(tb + 1) * (512 // r)]

            def nat_view(ap2d, r):
                if r == 1:
                    return ap2d
                return ap2d.rearrange("p (u r) -> p r u", r=r)

            def c_stage0(blk):
                if blk["tb"] == 0:
                    blk["Wd"]["W"] = nextW(blk["c"])
                W = blk["Wd"]["W"]
                bk = cnt["proj"] % 2
                cnt["proj"] += 1
                blk["bk"] = bk
                tb = blk["tb"]
                for kc in range(8):
                    mm(PS[bk].ap, W.ap[:, kc, :], xT.ap[:, kc, tb * 512:(tb + 1) * 512],
                       kc == 0, kc == 7, [W, xT], PS[bk], kc == 7)

            def c_stage1(blk):
                bk, tb, c, r, dst = blk["bk"], blk["tb"], blk["c"], blk["r"], blk["dst"]
                if blk["kind"] == 2:
                    K.op("act", lambda e: e.activation(
                        out=sub_view(dst.ap, r, tb), in_=nat_view(PS[bk].ap, r), func=AF.Identity,
                        bias=binT.ap[:, c:c + 1], scale=1.0), [PS[bk], binT], [dst])
                    return
                j = cnt["rot"] % 2
                cnt["rot"] += 1
                qb, t1, t2 = qb_t[j], t1_t[j], t2_t[j]
                K.op("act", lambda e: e.activation(out=qb.ap, in_=PS[bk].ap, func=AF.Identity,
                                                   bias=binT.ap[:, c:c + 1], scale=1.0),
                     [PS[bk], binT], [qb])
                blk["rot"] = (j, qb, t1, t2)

            def c_stage2(blk):
                if blk["kind"] == 2:
                    return
                j, qb, t1, t2 = blk["rot"]
                tb, r, dst = blk["tb"], blk["r"], blk["dst"]
                rb = 2 + j
                mm(PS[rb].ap, perm, qb.ap, True, True, [cbf, qb], PS[rb], True)
                K.op("dve", lambda e: e.tensor_tensor(out=t1.ap, in0=qb.ap,
                                                      in1=cosT.ap[:, tb * 512:(tb + 1) * 512],
                                                      op=ALU.mult), [qb, cosT], [t1])
                K.op("dve", lambda e: e.tensor_tensor(out=t2.ap, in0=PS[rb].ap,
                                                      in1=sinS.ap[:, tb * 512:(tb + 1) * 512],
                                                      op=ALU.mult), [PS[rb], sinS], [t2])
                K.op("pool", lambda e: e.tensor_tensor(out=sub_view(dst.ap, r, tb),
                                                       in0=nat_view(t1.ap, r),
                                                       in1=nat_view(t2.ap, r), op=ALU.add),
                     [t1, t2], [dst])

            def c_proj(it):
                issue_precast(4)
                h, g = it // 3, it % 3
                r = DIL[g]
                blks = []
                for kind in range(3):
                    Wd = {}
                    for tb in range(NTB):
                        blks.append({"kind": kind, "c": kind * 12 + g * 4 + h, "tb": tb, "r": r,
                                     "dst": qkv[it % 2][kind], "Wd": Wd})
                n = len(blks)
                for i in range(n + 2):
                    if i < n:
                        c_stage0(blks[i])
                    if 1 <= i <= n:
                        c_stage1(blks[i - 1])
                    if i >= 2:
                        c_stage2(blks[i - 2])

            def c_attn(it):
                h, g = it // 3, it % 3
                r = DIL[g]
                L = S // r
                nbk = L // 128
                qT_, kT_, vT_ = qkv[it % 2]
                vp = Vp[it % 2]
                for half in range(2):
                    bk = 2 + half
                    for jj in range(8):
                        jb = half * 8 + jj
                        transpose_to(bk, jj * 128, vT_.ap[:, jb * 128:(jb + 1) * 128], [vT_], jj == 7)
                    copy_any(vp.ap[:, half * 8:(half + 1) * 8, :],
                             psb(bk).rearrange("p (a b) -> p a b", a=8), [PS[bk]], [vp])

                def s_stage(pr):
                    sb = 4 + pr % 2
                    for bi, jb in enumerate((2 * pr, 2 * pr + 1)):
                        n = jb % nbk
                        jp = jb - 1 if n > 0 else jb
                        mk = m_cp if n > 0 else m_ca
                        qs = qT_.ap[:, jb * 128:(jb + 1) * 128]
                        c0 = bi * 256
                        mm(PS[sb].ap[:, c0:c0 + 128], kT_.ap[:, jb * 128:(jb + 1) * 128], qs,
                           bi == 0, False, [kT_, qT_], PS[sb], False, skip=True)
                        mm(PS[sb].ap[:, c0 + 128:c0 + 256], kT_.ap[:, jp * 128:(jp + 1) * 128], qs,
                           False, False, [kT_, qT_], PS[sb], False, skip=True)
                        mm(PS[sb].ap[:, c0:c0 + 256], ident, mk, False, True, [cbf], PS[sb], bi == 1, skip=True)

                def e_stage(pr):
                    sb = 4 + pr % 2
                    pT = pT_t[pr % 2]
                    K.op("act", lambda e: e.activation(out=pT.ap, in_=PS[sb].ap, func=AF.Exp, scale=SCALE),
                         [PS[sb]], [pT])

                def p_stage(pr):
                    ob = 6 + pr % 2
                    pT = pT_t[pr % 2]
                    blocks = (2 * pr, 2 * pr + 1)
                    for bi, jb in enumerate(blocks):
                        n = jb % nbk
                        jp = jb - 1 if n > 0 else jb
                        c0 = bi * 256
                        o_ap = PS[ob].ap[:, bi * 128:(bi + 1) * 128]
                        d_ap = PS[ob].ap[:, 256 + bi * 128:256 + (bi + 1) * 128]
                        mm(o_ap, vp.ap[:, jb, :], pT.ap[:, c0:c0 + 128], bi == 0, False, [vp, pT], PS[ob], False,
                           skip=True)
                        mm(o_ap, vp.ap[:, jp, :], pT.ap[:, c0 + 128:c0 + 256], False, True, [vp, pT],
                           PS[ob], False, skip=True)

                    pT3 = pT.ap.rearrange("p (b c q) -> p b c q", b=2, c=2)
                    d2 = PS[ob].ap[:, 256:512].rearrange("p (b q) -> p b q", b=2)
                    mm(d2, onesb, pT3[:, :, 0, :], False, False, [cbf, pT], PS[ob], False, skip=True)
                    mm(d2, onesb, pT3[:, :, 1, :], False, True, [cbf, pT], PS[ob], True, skip=True)
                    jb0 = blocks[0]
                    if nbk >= 2:
                        res_, n0 = jb0 // nbk, jb0 % nbk
                        if r == 1:
                            dO = accO.ap[:, n0 * 128:n0 * 128 + 256]
                            dD = accD.ap[:, n0 * 128:n0 * 128 + 256]
                        else:
                            dO = accO.ap.rearrange("p (u r) -> p r u", r=r)[:, res_, n0 * 128:n0 * 128 + 256]
                            dD = accD.ap.rearrange("p (u r) -> p r u", r=r)[:, res_, n0 * 128:n0 * 128 + 256]
                        sO = PS[ob].ap[:, 0:256]
                        sD = PS[ob].ap[:, 256:512]
                    else:
                        dO = accO.ap.rearrange("p (u r) -> p r u", r=r)[:, jb0:jb0 + 2, :]
                        dD = accD.ap.rearrange("p (u r) -> p r u", r=r)[:, jb0:jb0 + 2, :]
                        sO = PS[ob].ap[:, 0:256].rearrange("p (a b) -> p a b", a=2)
                        sD = PS[ob].ap[:, 256:512].rearrange("p (a b) -> p a b", a=2)
                    if g == 0:
                        K.op("act", lambda e: e.copy(out=dO, in_=sO), [PS[ob]], [accO])
                        K.op("act", lambda e: e.copy(out=dD, in_=sD), [PS[ob]], [accD])
                    else:
                        K.op("dve", lambda e: e.tensor_tensor(out=dO, in0=sO, in1=dO, op=ALU.add),
                             [PS[ob], accO], [accO])
                        K.op("dve", lambda e: e.tensor_tensor(out=dD, in0=sD, in1=dD, op=ALU.add),
                             [PS[ob], accD], [accD])

                s_stage(0)
                for pr in range(8):
                    if pr + 1 < 8:
                        s_stage(pr + 1)
                    e_stage(pr)
                    p_stage(pr)
                if g == 2:
                    K.op("dve", lambda e: e.reciprocal(out=accD.ap, in_=accD.ap), [accD], [accD])
                    K.op("pool", lambda e: e.tensor_tensor(out=oT.ap[:, h, :], in0=accO.ap, in1=accD.ap,
                                                           op=ALU.mult), [accO, accD], [oT])

            NIT = 12
            c_proj(0)
            for it in range(NIT):
                if it + 1 < NIT:
                    c_proj(it + 1)
                c_attn(it)
            K.barrier()
            A.release(pa_mark)
            if stage == "C":
                raise _Stop()
            cT = A.alloc("cT", [128, 4, S + 32], BF16)
            dg = A.alloc("dg", [128, 4 * 31, 128], BF16)
            ga = [A.alloc("ga%d" % i, [128, 512], F32) for i in range(2)]
            gs = [A.alloc("gs%d" % i, [128, 512], F32) for i in range(2)]
            zf = A.alloc("zf", [128, 4, 512], F32)
            zq = A.alloc("zq", [128, 4, 512], F32)
            mS = A.alloc("mS", [128, 512], F32)
            rS = A.alloc("rS", [128, 512], F32)
            K.op("pool", lambda e: e.memset(cT.ap[:, :, 0:32], 0.0), [], [cT])
            dg_todo = [(ch, j) for ch in range(4) for j in range(31)]

            def build_dg(n):
                for _ in range(n):
                    if dg_todo:
                        ch_, j_ = dg_todo.pop(0)
                        K.op("dve", lambda e: e.tensor_scalar(out=dg.ap[:, ch_ * 31 + j_, :], in0=ident,
                                                              scalar1=convw.ap[:, ch_, j_:j_ + 1], scalar2=None,
                                                              op0=ALU.mult), [cbf, convw], [dg])
            for ch in range(4):
                Wa = getW(36 + ch)
                Wb = getW(40 + ch)
                for tb in range(NTB):
                    j = (ch * NTB + tb) % 2
                    for W, bk, dst, fn, c in ((Wa, 2 * j, ga[j], AF.Identity, 36 + ch),
                                              (Wb, 2 * j + 1, gs[j], AF.Sigmoid, 40 + ch)):
                        for kc in range(8):
                            mm(PS[bk].ap, W.ap[:, kc, :], xT.ap[:, kc, tb * 512:(tb + 1) * 512],
                               kc == 0, kc == 7, [W, xT], PS[bk], kc == 7)
                        K.op("act", lambda e: e.activation(out=dst.ap, in_=PS[bk].ap, func=fn,
                                                           bias=binT.ap[:, c:c + 1], scale=1.0),
                             [PS[bk], binT], [dst])
                    K.op("dve", lambda e: e.tensor_tensor(out=cT.ap[:, ch, 32 + tb * 512:32 + (tb + 1) * 512],
                                                          in0=ga[j].ap, in1=gs[j].ap, op=ALU.mult),
                         [ga[j], gs[j]], [cT])
                    build_dg(8)
            for tb in range(NTB):
                for ch in range(4):
                    bk = 2 + ch
                    for j in range(31):
                        c0 = 2 + tb * 512 + j
                        mm(PS[bk].ap, dg.ap[:, ch * 31 + j, :], cT.ap[:, ch, c0:c0 + 512],
                           j == 0, j == 30, [dg, cT], PS[bk], j == 30)
                    K.op("act", lambda e: e.activation(out=zf.ap[:, ch, :], in_=PS[bk].ap, func=AF.Identity,
                                                       bias=convp.ap[:, 0, ch:ch + 1], scale=1.0),
                         [PS[bk], convp], [zf])
                    K.op("act", lambda e: e.activation(out=zq.ap[:, ch, :], in_=PS[bk].ap, func=AF.Square,
                                                       bias=convp.ap[:, 0, ch:ch + 1], scale=1.0),
                         [PS[bk], convp], [zq])
                for ch in range(4):
                    mm(PS[6].ap, onesf.ap, zf.ap[:, ch, :], ch == 0, ch == 3, [onesf, zf], PS[6], ch == 3)
                for ch in range(4):
                    mm(PS[7].ap, onesf.ap, zq.ap[:, ch, :], ch == 0, ch == 3, [onesf, zq], PS[7], ch == 3)
                K.op("act", lambda e: e.copy(out=mS.ap, in_=PS[6].ap), [PS[6]], [mS])
                K.op("dve", lambda e: e.tensor_tensor(out=rS.ap, in0=mS.ap, in1=mS.ap, op=ALU.mult), [mS], [rS])
                K.op("dve", lambda e: e.tensor_tensor(out=rS.ap, in0=PS[7].ap, in1=rS.ap, op=ALU.subtract),
                     [PS[7], rS], [rS])
                K.op("act", lambda e: e.activation(out=rS.ap, in_=rS.ap, func=AF.Sqrt, bias=EPS, scale=1.0),
                     [rS], [rS])
                K.op("dve", lambda e: e.reciprocal(out=rS.ap, in_=rS.ap), [rS], [rS])
                for ch in range(4):
                    K.op("dve", lambda e: e.tensor_tensor(out=zf.ap[:, ch, :], in0=zf.ap[:, ch, :], in1=mS.ap,
                                                          op=ALU.subtract), [zf, mS], [zf])
                    K.op("pool", lambda e: e.tensor_tensor(out=zf.ap[:, ch, :], in0=zf.ap[:, ch, :], in1=rS.ap,
                                                           op=ALU.mult), [zf, rS], [zf])
                    K.op("act", lambda e: e.activation(out=cnT.ap[:, ch, tb * 512:(tb + 1) * 512],
                                                       in_=zf.ap[:, ch, :], func=AF.Silu,
                                                       bias=convp.ap[:, 2, ch:ch + 1],
                                                       scale=convp.ap[:, 1, ch:ch + 1]), [zf, convp], [cnT])
            K.barrier()
            A.release(pa_mark)
            if stage == "D":
                raise _Stop()
            qm_t = [A.alloc("qm%d" % i, [128, 512], BF16) for i in range(2)]
            pm_t = [A.alloc("pm%d" % i, [128, 2, 512], BF16) for i in range(2)]
            rd_t = [A.alloc("rd%d" % i, [128, 512], F32) for i in range(2)]
            Wm = {}

            def e_s0(q):
                hm, tb = q // NTB, q % NTB
                if tb == 0:
                    Wm[hm] = getW(44 + hm)
                W = Wm[hm]
                bk = q % 2
                for kc in range(8):
                    mm(PS[bk].ap, W.ap[:, kc, :], xT.ap[:, kc, tb * 512:(tb + 1) * 512],
                       kc == 0, kc == 7, [W, xT], PS[bk], kc == 7)

            def e_s1(q):
                hm = q // NTB
                j = q % 2
                qm = qm_t[j]
                c = 44 + hm
                K.op("act", lambda e: e.activation(out=qm.ap, in_=PS[j].ap, func=AF.Identity,
                                                   bias=binT.ap[:, c:c + 1], scale=1.0), [PS[j], binT], [qm])
                for mc in range(2):
                    sbk = 2 + 2 * j + mc
                    mm(PS[sbk].ap, kmT.ap[:, hm, mc * 128:(mc + 1) * 128], qm.ap, True, True, [kmT, qm],
                       PS[sbk], True)

            def e_s2(q):
                j = q % 2
                pm = pm_t[j]
                for mc in range(2):
                    sbk = 2 + 2 * j + mc
                    K.op("act", lambda e: e.activation(out=pm.ap[:, mc, :], in_=PS[sbk].ap, func=AF.Exp,
                                                       scale=SCALE), [PS[sbk]], [pm])

            def e_s3(q):
                hm = q // NTB
                pm = pm_t[q % 2]
                for mc in range(2):
                    mm(PS[6].ap, vm.ap[:, mc, hm * 128:(hm + 1) * 128], pm.ap[:, mc, :], mc == 0, mc == 1,
                       [vm, pm], PS[6], mc == 1)
                for mc in range(2):
                    mm(PS[7].ap, onesb, pm.ap[:, mc, :], mc == 0, mc == 1, [cbf, pm], PS[7], mc == 1)

            def e_s4(q):
                hm, tb = q // NTB, q % NTB
                rd = rd_t[q % 2]
                K.op("dve", lambda e: e.reciprocal(out=rd.ap, in_=PS[7].ap), [PS[7]], [rd])
                K.op("dve", lambda e: e.tensor_tensor(out=omT.ap[:, hm, tb * 512:(tb + 1) * 512],
                                                      in0=PS[6].ap, in1=rd.ap, op=ALU.mult),
                     [PS[6], rd], [omT])

            NQ = 4 * NTB
            stages = [(e_s4, 4), (e_s3, 3), (e_s2, 2), (e_s1, 1), (e_s0, 0)]
            for i in range(NQ + 4):
                for fn_, d_ in stages:
                    q_ = i - d_
                    if 0 <= q_ < NQ:
                        fn_(q_)
            K.barrier()
            A.release(o_mark)
            if stage == "E":
                raise _Stop()
            wo3 = [A.alloc("wo%d" % i, [128, 4, 1024], BF16) for i in range(3)]
            for w_, d_ in zip(wo3, (wao_d, wco_d, wmo_d)):
                K.dma("pool", [(w_.ap, d_)], w_, writes=[w_])
            mergedT0 = A.alloc("mergedT0", [128, 8, S], BF16)
            mergedT = mergedT0
            gt = [[A.alloc("gt%d_%d" % (i, j), [128, 512], F32) for j in range(3)] for i in range(2)]
            mt = [[A.alloc("mt%d_%d" % (i, j), [128, 512], F32) for j in range(3)] for i in range(2)]
            srcs = (oT, cnT, omT)
            it = 0
            pj = 0
            for dc in range(8):
                Ws = [getW(48 + br * 8 + dc) for br in range(3)]
                for tb in range(NTB):
                    j = it % 2
                    it += 1
                    for br in range(3):
                        bk = pj % 4
                        pj += 1
                        c = 48 + br * 8 + dc
                        for kc in range(8):
                            mm(PS[bk].ap, Ws[br].ap[:, kc, :], xT.ap[:, kc, tb * 512:(tb + 1) * 512],
                               kc == 0, kc == 7, [Ws[br], xT], PS[bk], kc == 7)
                        g_ = gt[j][br]
                        K.op("act", lambda e: e.activation(out=g_.ap, in_=PS[bk].ap, func=AF.Sigmoid,
                                                           bias=binT.ap[:, c:c + 1], scale=1.0),
                             [PS[bk], binT], [g_])
                        yb = 4 + (it * 3 + br) % 4
                        for hc in range(4):
                            mm(PS[yb].ap, wo3[br].ap[:, hc, dc * 128:(dc + 1) * 128],
                               srcs[br].ap[:, hc, tb * 512:(tb + 1) * 512], hc == 0, hc == 3,
                               [wo3[br], srcs[br]], PS[yb], hc == 3)
                        m_ = mt[j][br]
                        K.op("dve", lambda e: e.tensor_tensor(out=m_.ap, in0=PS[yb].ap, in1=g_.ap, op=ALU.mult),
                             [PS[yb], g_], [m_])
                    K.op("pool", lambda e: e.tensor_tensor(out=mt[j][0].ap, in0=mt[j][0].ap, in1=mt[j][1].ap,
                                                           op=ALU.add), [mt[j][0], mt[j][1]], [mt[j][0]])
                    K.op("pool", lambda e: e.tensor_tensor(out=mergedT.ap[:, dc, tb * 512:(tb + 1) * 512],
                                                           in0=mt[j][0].ap, in1=mt[j][2].ap, op=ALU.add),
                         [mt[j][0], mt[j][2]], [mergedT])
            K.barrier()
            A.release(base_mark)
            if stage == "F1":
                raise _Stop()
            mergedT = A.alloc("mergedT", [128, 8, S], BF16)
            wout = A.alloc("wout", [128, 8, 1024], BF16)
            K.dma("pool", [(wout.ap, wout_d)], wout, writes=[wout])
            K.op("dve", lambda e: e.tensor_copy(out=mergedT.ap[:, 0:4, :], in_=mergedT0.ap[:, 0:4, :]),
                 [mergedT0], [mergedT])
            K.op("act", lambda e: e.copy(out=mergedT.ap[:, 4:6, :], in_=mergedT0.ap[:, 4:6, :]),
                 [mergedT0], [mergedT])
            K.op("dve", lambda e: e.tensor_copy(out=mergedT.ap[:, 6:8, :], in_=mergedT0.ap[:, 6:8, :]),
                 [mergedT0], [mergedT])
            K.barrier()
            x1T = A.alloc("x1T", [128, 8, S], BF16, top=True)
            gate = A.alloc("gate", [128, NT, 16], F32, top=True)
            oha = A.alloc("oha", [128, NT, 16], F32, top=True)
            ohb = A.alloc("ohb", [128, NT, 16], F32, top=True)
            wts = A.alloc("wts", [128, 2, NT], F32, top=True)
            xr = [A.alloc("xr%d" % i, [128, D], F32) for i in range(4)]
            vt = [A.alloc("vt%d" % i, [128, D], F32) for i in range(4)]
            tmpn = [A.alloc("tmpn%d" % i, [128, D], F32) for i in range(4)]
            x1t = [A.alloc("x1t%d" % i, [128, D], F32) for i in range(4)]
            xhi = [A.alloc("xhi%d" % i, [128, D], BF16) for i in range(4)]
            xlo = [A.alloc("xlo%d" % i, [128, D], BF16) for i in range(4)]
            xloT = [A.alloc("xloT%d" % i, [128, 8, 128], BF16) for i in range(4)]
            stt = [A.alloc("st%d" % i, [128, 16], F32) for i in range(4)]
            lg = A.alloc("lg", [128, NT, 20], F32)
            def f2_ldx(tt):
                j = tt % 4
                K.dma("sp", [(xr[j].ap, x_d[b, tt * 128:(tt + 1) * 128, :])], xr[j], writes=[xr[j]])

            def f2_mix(tt):
                j = tt % 4
                for dh in range(2):
                    bk = 2 * (tt % 2) + dh
                    for kc in range(8):
                        mm(PS[bk].ap, mergedT.ap[:, kc, tt * 128:(tt + 1) * 128],
                           wout.ap[:, kc, dh * 512:(dh + 1) * 512], kc == 0, kc == 7, [mergedT, wout], PS[bk],
                           kc == 7)
                    K.op("dve", lambda e: e.scalar_tensor_tensor(
                        out=vt[j].ap[:, dh * 512:(dh + 1) * 512], in0=xr[j].ap[:, dh * 512:(dh + 1) * 512],
                        scalar=ALPHA, in1=PS[bk].ap, op0=ALU.mult, op1=ALU.add), [xr[j], PS[bk]], [vt[j]])

            def f2_s1(tt):
                ln_rstd(stt[tt % 4])

            def f2_s2(tt):
                j = tt % 4
                ln_norm(vt[j], 0, x1t[j], tmpn[j], stt[j])

            def f2_s3(tt):
                j = tt % 4
                K.op("act", lambda e: e.copy(out=xhi[j].ap, in_=x1t[j].ap), [x1t[j]], [xhi[j]])
                K.op("pool", lambda e: e.tensor_tensor(out=xlo[j].ap, in0=x1t[j].ap, in1=xhi[j].ap,
                                                       op=ALU.subtract), [x1t[j], xhi[j]], [xlo[j]])
                if SPARSE:
                    K.dma("sp", [(x1h_d[b, tt * 128:(tt + 1) * 128, :], xhi[j].ap)], xhi[j],
                          reads=[xhi[j]], writes=[x1h_res[b][tt]])
                K.op("act", lambda e: e.activation(out=tmpn[j].ap, in_=x1t[j].ap, func=AF.Copy, scale=ALPHA),
                     [x1t[j]], [tmpn[j]])
                K.dma("sp", [(x1s_d[b, tt * 128:(tt + 1) * 128, :], tmpn[j].ap)], tmpn[j],
                      reads=[tmpn[j]], writes=[x1s_res[b][tt]])

            def f2_s4(tt):
                j = tt % 4
                bh = 4 + tt % 2
                for kc in range(8):
                    transpose_to(bh, kc * 128, xhi[j].ap[:, kc * 128:(kc + 1) * 128], [xhi[j]], kc == 7)
                bl = 6
                for kc in range(8):
                    transpose_to(bl, kc * 128, xlo[j].ap[:, kc * 128:(kc + 1) * 128], [xlo[j]], kc == 7)

            def f2_s5(tt):
                j = tt % 4
                bh = 4 + tt % 2
                bl = 6
                K.op("act", lambda e: e.copy(out=x1T.ap[:, :, tt * 128:(tt + 1) * 128],
                                             in_=psb(bh).rearrange("p (a b) -> p a b", a=8)), [PS[bh]], [x1T])
                K.op("dve", lambda e: e.tensor_copy(out=xloT[j].ap,
                                                    in_=psb(bl).rearrange("p (a b) -> p a b", a=8)),
                     [PS[bl]], [xloT[j]])

            def f2_s6(tt):
                j = tt % 4
                terms = []
                for kc in range(8):
                    terms.append((x1T.ap[:, kc, tt * 128:(tt + 1) * 128], wr_hi.ap[:, kc, :]))
                    terms.append((xloT[j].ap[:, kc, :], wr_hi.ap[:, kc, :]))
                    terms.append((x1T.ap[:, kc, tt * 128:(tt + 1) * 128], wr_lo.ap[:, kc, :]))
                for ti, (l_, r_) in enumerate(terms):
                    mm(PS[7].ap[:, 0:20], l_, r_, ti == 0, ti == len(terms) - 1, [x1T, xloT[j], wr_hi, wr_lo],
                       PS[7], ti == len(terms) - 1)
                K.op("dve", lambda e: e.tensor_tensor(out=lg.ap[:, tt, :], in0=PS[7].ap[:, 0:20], in1=brb.ap,
                                                      op=ALU.add), [PS[7], brb], [lg])

            stages = [f2_s6, f2_s5, f2_s4, f2_s3, f2_s2, f2_s1]
            nst = len(stages)
            for i in range(NT + nst):
                for k, fn_ in enumerate(stages):
                    tt_ = i - (nst - k)
                    if 0 <= tt_ < NT:
                        fn_(tt_)
                if i == 0:
                    f2_ldx(0)
                    f2_ldx(1)
                if i + 2 < NT:
                    f2_ldx(i + 2)
                if i < NT:
                    f2_mix(i)
                    ln_stats(vt[i % 4], stt[i % 4])
            if stage == "F2":
                raise _Stop()
            r_ = {}
            for nm, shp in (("gmax", [128, NT]), ("ohg", [128, NT, 4]), ("dg_", [128, NT, 4]),
                            ("gsum", [128, NT]), ("tmp4", [128, NT, 4, 4]), ("els", [128, NT, 4]),
                            ("m1", [128, NT]), ("oh1", [128, NT, 4]), ("msk", [128, NT, 4]),
                            ("m2", [128, NT]), ("oh2", [128, NT, 4]), ("e2", [128, NT]), ("w1", [128, NT]),
                            ("w2", [128, NT]), ("ew", [128, NT, 4]), ("ew2", [128, NT, 4])):
                r_[nm] = A.alloc("r_" + nm, shp, F32)
            gl = lg.ap[:, :, 0:4]
            el = lg.ap[:, :, 4:20].rearrange("p t (g e) -> p t g e", g=4)

            def bc3(t2):
                return t2.unsqueeze(2).broadcast_to([128, NT, 4])

            def dv(fn, reads, writes):
                K.op("dve", fn, reads, writes)
            R = r_
            dv(lambda e: e.tensor_reduce(out=R["gmax"].ap, in_=gl, axis=AX.X, op=ALU.max), [lg], [R["gmax"]])
            dv(lambda e: e.tensor_tensor(out=R["ohg"].ap, in0=gl, in1=bc3(R["gmax"].ap), op=ALU.is_equal),
               [lg, R["gmax"]], [R["ohg"]])
            dv(lambda e: e.tensor_tensor(out=R["dg_"].ap, in0=gl, in1=bc3(R["gmax"].ap), op=ALU.subtract),
               [lg, R["gmax"]], [R["dg_"]])
            K.op("act", lambda e: e.activation(out=R["dg_"].ap, in_=R["dg_"].ap, func=AF.Exp), [R["dg_"]],
                 [R["dg_"]])
            dv(lambda e: e.tensor_reduce(out=R["gsum"].ap, in_=R["dg_"].ap, axis=AX.X, op=ALU.add), [R["dg_"]],
               [R["gsum"]])
            dv(lambda e: e.reciprocal(out=R["gsum"].ap, in_=R["gsum"].ap), [R["gsum"]], [R["gsum"]])
            dv(lambda e: e.tensor_tensor(out=R["tmp4"].ap, in0=el,
                                         in1=R["ohg"].ap.unsqueeze(3).broadcast_to([128, NT, 4, 4]),
                                         op=ALU.mult), [lg, R["ohg"]], [R["tmp4"]])
            dv(lambda e: e.tensor_reduce(out=R["els"].ap, in_=R["tmp4"].ap.rearrange("p t g e -> p t e g"),
                                         axis=AX.X, op=ALU.add), [R["tmp4"]], [R["els"]])
            dv(lambda e: e.tensor_reduce(out=R["m1"].ap, in_=R["els"].ap, axis=AX.X, op=ALU.max), [R["els"]],
               [R["m1"]])
            dv(lambda e: e.tensor_tensor(out=R["oh1"].ap, in0=R["els"].ap, in1=bc3(R["m1"].ap), op=ALU.is_equal),
               [R["els"], R["m1"]], [R["oh1"]])
            dv(lambda e: e.scalar_tensor_tensor(out=R["msk"].ap, in0=R["oh1"].ap, scalar=-1e30, in1=R["els"].ap,
                                                op0=ALU.mult, op1=ALU.add), [R["oh1"], R["els"]], [R["msk"]])
            dv(lambda e: e.tensor_reduce(out=R["m2"].ap, in_=R["msk"].ap, axis=AX.X, op=ALU.max), [R["msk"]],
               [R["m2"]])
            dv(lambda e: e.tensor_tensor(out=R["oh2"].ap, in0=R["msk"].ap, in1=bc3(R["m2"].ap), op=ALU.is_equal),
               [R["msk"], R["m2"]], [R["oh2"]])
            dv(lambda e: e.tensor_tensor(out=R["e2"].ap, in0=R["m2"].ap, in1=R["m1"].ap, op=ALU.subtract),
               [R["m2"], R["m1"]], [R["e2"]])
            K.op("act", lambda e: e.activation(out=R["e2"].ap, in_=R["e2"].ap, func=AF.Exp), [R["e2"]], [R["e2"]])
            dv(lambda e: e.tensor_scalar(out=R["w1"].ap, in0=R["e2"].ap, scalar1=1.0, scalar2=None, op0=ALU.add),
               [R["e2"]], [R["w1"]])
            dv(lambda e: e.reciprocal(out=R["w1"].ap, in_=R["w1"].ap), [R["w1"]], [R["w1"]])
            dv(lambda e: e.tensor_tensor(out=R["w2"].ap, in0=R["e2"].ap, in1=R["w1"].ap, op=ALU.mult),
               [R["e2"], R["w1"]], [R["w2"]])
            dv(lambda e: e.tensor_tensor(out=R["w1"].ap, in0=R["w1"].ap, in1=R["gsum"].ap, op=ALU.mult),
               [R["w1"], R["gsum"]], [R["w1"]])
            dv(lambda e: e.tensor_tensor(out=R["w2"].ap, in0=R["w2"].ap, in1=R["gsum"].ap, op=ALU.mult),
               [R["w2"], R["gsum"]], [R["w2"]])
            if SPARSE:
                dv(lambda e: e.tensor_copy(out=wts.ap[:, 0, :], in_=R["w1"].ap), [R["w1"]], [wts])
                dv(lambda e: e.tensor_copy(out=wts.ap[:, 1, :], in_=R["w2"].ap), [R["w2"]], [wts])
                for oh_, o_ in ((R["oh1"], oha), (R["oh2"], ohb)):
                    dv(lambda e: e.tensor_tensor(out=o_.ap.rearrange("p t (g e) -> p t g e", g=4),
                                                 in0=R["ohg"].ap.unsqueeze(3).broadcast_to([128, NT, 4, 4]),
                                                 in1=oh_.ap.unsqueeze(2).broadcast_to([128, NT, 4, 4]),
                                                 op=ALU.mult), [R["ohg"], oh_], [o_])
            dv(lambda e: e.tensor_tensor(out=R["ew"].ap, in0=R["oh1"].ap, in1=bc3(R["w1"].ap), op=ALU.mult),
               [R["oh1"], R["w1"]], [R["ew"]])
            dv(lambda e: e.tensor_tensor(out=R["ew2"].ap, in0=R["oh2"].ap, in1=bc3(R["w2"].ap), op=ALU.mult),
               [R["oh2"], R["w2"]], [R["ew2"]])
            dv(lambda e: e.tensor_tensor(out=R["ew"].ap, in0=R["ew"].ap, in1=R["ew2"].ap, op=ALU.add),
               [R["ew"], R["ew2"]], [R["ew"]])
            dv(lambda e: e.tensor_tensor(out=gate.ap.rearrange("p t (g e) -> p t g e", g=4),
                                         in0=R["ohg"].ap.unsqueeze(3).broadcast_to([128, NT, 4, 4]),
                                         in1=R["ew"].ap.unsqueeze(2).broadcast_to([128, NT, 4, 4]),
                                         op=ALU.mult), [R["ohg"], R["ew"]], [gate])
            K.barrier()
            A.release(base_mark)
            if stage == "R":
                raise _Stop()
            if SPARSE:
                sparse_moe(b, oha, ohb, wts)
                continue
            yacc = A.alloc("yacc", [128, NT, D], F32)
            moe_w_mark = A.mark()
            ew_ = [[A.alloc("wg%d" % i, [128, 8, 512], BF16), A.alloc("wu%d" % i, [128, 8, 512], BF16),
                    A.alloc("wd%d" % i, [128, 4, 1024], BF16)] for i in range(2)]
            hid = [A.alloc("hid%d" % i, [128, 4, 512], BF16) for i in range(2)]
            sg = [A.alloc("sg%d" % i, [128, 512], F32) for i in range(2)]

            def load_expert(e_):
                s_ = ew_[e_ % 2]
                for t_, d_ in zip(s_, (wg_d, wu_d, wd_d)):
                    K.dma("pool", [(t_.ap, d_[e_])], t_, writes=[t_])
            load_expert(0)
            gu = 0
            yb_ = 0
            for ex in range(16):
                if ex + 1 < 16:
                    load_expert(ex + 1)
                wg_, wu_, wd_ = ew_[ex % 2]
                for tb in range(NTB):
                    hd = hid[(ex * NTB + tb) % 2]
                    for fc in range(4):
                        bg = 2 * (gu % 2)
                        bu = bg + 1
                        gu += 1
                        for kc in range(8):
                            mm(PS[bg].ap, wg_.ap[:, kc, fc * 128:(fc + 1) * 128],
                               x1T.ap[:, kc, tb * 512:(tb + 1) * 512], kc == 0, kc == 7, [wg_, x1T], PS[bg],
                               kc == 7)
                        for kc in range(8):
                            mm(PS[bu].ap, wu_.ap[:, kc, fc * 128:(fc + 1) * 128],
                               x1T.ap[:, kc, tb * 512:(tb + 1) * 512], kc == 0, kc == 7, [wu_, x1T], PS[bu],
                               kc == 7)
                        s_ = sg[gu % 2]
                        K.op("act", lambda e: e.activation(out=s_.ap, in_=PS[bg].ap, func=AF.Silu), [PS[bg]], [s_])
                        K.op("dve", lambda e: e.tensor_tensor(out=hd.ap[:, fc, :], in0=PS[bu].ap, in1=s_.ap,
                                                              op=ALU.mult), [PS[bu], s_], [hd])
                    for ti in range(4):
                        tt = tb * 4 + ti
                        for dh in range(2):
                            by = 4 + (yb_ % 4)
                            yb_ += 1
                            for fc in range(4):
                                mm(PS[by].ap, hd.ap[:, fc, ti * 128:(ti + 1) * 128],
                                   wd_.ap[:, fc, dh * 512:(dh + 1) * 512], fc == 0, fc == 3, [hd, wd_], PS[by],
                                   fc == 3)
                            ya = yacc.ap[:, tt, dh * 512:(dh + 1) * 512]
                            gsc = gate.ap[:, tt, ex:ex + 1]
                            if ex == 0:
                                K.op("dve", lambda e: e.tensor_scalar(out=ya, in0=PS[by].ap, scalar1=gsc,
                                                                      scalar2=None, op0=ALU.mult),
                                     [PS[by], gate], [yacc])
                            else:
                                K.op("dve", lambda e: e.scalar_tensor_tensor(out=ya, in0=PS[by].ap, scalar=gsc,
                                                                             in1=ya, op0=ALU.mult, op1=ALU.add),
                                     [PS[by], gate, yacc], [yacc])
            K.barrier()
            A.release(moe_w_mark)
            xr2 = [A.alloc("xr2_%d" % i, [128, D], F32) for i in range(4)]
            tmp2 = [A.alloc("tmp2_%d" % i, [128, D], F32) for i in range(2)]
            ot = [A.alloc("ot%d" % i, [128, D], F32) for i in range(4)]
            st2 = [A.alloc("st2_%d" % i, [128, 16], F32) for i in range(4)]

            def l2_a(tt):
                j = tt % 4
                K.dma("sp", [(xr2[j].ap, x1s_d[b, tt * 128:(tt + 1) * 128, :])], xr2[j],
                      reads=[x1s_res[b][tt]], writes=[xr2[j]])

            def l2_b(tt):
                j = tt % 4
                K.op("dve", lambda e: e.scalar_tensor_tensor(out=xr2[j].ap, in0=xr2[j].ap, scalar=ALPHA,
                                                             in1=yacc.ap[:, tt, :], op0=ALU.mult, op1=ALU.add),
                     [xr2[j], yacc], [xr2[j]])
                ln_stats(xr2[j], st2[j])

            def l2_c(tt):
                ln_rstd(st2[tt % 4])

            def l2_d(tt):
                j = tt % 4
                ln_norm(xr2[j], 2, ot[j], tmp2[tt % 2], st2[j])

            def l2_e(tt):
                j = tt % 4
                K.dma("sp", [(out_d[b, tt * 128:(tt + 1) * 128, :], ot[j].ap)], ot[j], reads=[ot[j]])

            stages = [l2_e, l2_d, l2_c, l2_b]
            nst = len(stages)
            for i in range(NT + nst):
                for k, fn_ in enumerate(stages):
                    tt_ = i - (nst - k)
                    if 0 <= tt_ < NT:
                        fn_(tt_)
                if i < NT:
                    l2_a(i)
          except _Stop:
            pass
        K.barrier()
        K.final_wait()
        nc._arena_where = A.where
    return nc


def _consts():
    ident = np.eye(128, dtype=np.float32)
    perm = np.zeros((128, 128), np.float32)
    for m in range(64):
        perm[m + 64, m] = 1.0
        perm[m, m + 64] = 1.0
    ones = np.ones((128, 128), np.float32)
    kk = np.arange(128)[:, None]
    qq = np.arange(128)[None, :]
    m_cur = np.where(qq >= kk, 0.0, NEG).astype(np.float32)
    m_prev = np.where(qq <= kk, 0.0, NEG).astype(np.float32)
    m_all = np.full((128, 128), NEG, np.float32)
    ustrict = (kk < qq).astype(np.float32)
    cbf = np.stack([ident, perm, ones, m_cur, m_prev, m_all, ustrict, m_cur, m_all], axis=1).astype(ml_dtypes.bfloat16)
    half = 64
    inv = (np.float32(10000.0) ** (-np.arange(half, dtype=np.float32) / np.float32(half))).astype(np.float32)
    cf = np.zeros((128, 4), np.float32)
    cf[:, 0] = np.concatenate([inv, inv])
    cf[:, 1] = np.concatenate([-np.ones(64), np.ones(64)])
    cf[:, 2] = np.arange(128)
    onesf = np.full((128, 128), 1.0 / 512.0, np.float32)
    return cbf, cf, onesf


def _prep_shared(w_in, b_in, conv_w, conv_b, conv_ln_g, conv_ln_b, w_mem_kv, w_attn_o, w_conv_o, w_mem_o,
                 w_out, ln1_g, ln1_b, w_group_router, b_group_router, w_expert_router, b_expert_router,
                 w_exp_gate, w_exp_up, w_exp_down, ln2_g, ln2_b):
    f = lambda a: np.ascontiguousarray(np.asarray(a, dtype=np.float32))
    cbf, cf, onesf = _consts()
    sh = {}
    wi = f(w_in)[0]
    sh["w_in_r"] = f(wi.reshape(8, 128, 72, 128).transpose(2, 1, 0, 3))
    sh["b_inT"] = f(f(b_in)[0].reshape(72, 128).T)
    sh["b_v"] = f(f(b_in)[0][3072:4608][None, :])
    sh["conv_wT"] = f(f(conv_w)[0].reshape(31, 4, 128).transpose(2, 1, 0))
    sh["conv_p"] = f(np.stack([f(conv_b)[0].reshape(4, 128).T, f(conv_ln_g)[0].reshape(4, 128).T,
                               f(conv_ln_b)[0].reshape(4, 128).T], axis=1))
    sh["w_kv_r"] = f(f(w_mem_kv)[0].reshape(8, 128, 1024).transpose(1, 0, 2))
    sh["w_ao_r"] = f(f(w_attn_o)[0].reshape(4, 128, 1024).transpose(1, 0, 2))
    sh["w_co_r"] = f(f(w_conv_o)[0].reshape(4, 128, 1024).transpose(1, 0, 2))
    sh["w_mo_r"] = f(f(w_mem_o)[0].reshape(4, 128, 1024).transpose(1, 0, 2))
    sh["w_out_r"] = f(f(w_out)[0].reshape(8, 128, 1024).transpose(1, 0, 2))
    sh["ln_p"] = f(np.stack([f(ln1_g)[0], f(ln1_b)[0], f(ln2_g)[0], f(ln2_b)[0]], axis=0))
    wr = np.concatenate([f(w_group_router)[0]] + [f(w_expert_router)[0][g] for g in range(4)], axis=1)
    sh["w_r_r"] = f(wr.reshape(8, 128, 20).transpose(1, 0, 2))
    sh["b_r"] = f(np.concatenate([f(b_group_router)[0], f(b_expert_router)[0].reshape(16)])[None, :])
    sh["w_g_r"] = f(f(w_exp_gate)[0].reshape(16, 8, 128, 512).transpose(0, 2, 1, 3))
    sh["w_u_r"] = f(f(w_exp_up)[0].reshape(16, 8, 128, 512).transpose(0, 2, 1, 3))
    sh["w_d_r"] = f(f(w_exp_down)[0].reshape(16, 4, 128, 1024).transpose(0, 2, 1, 3))
    sh["c_bf"] = cbf
    sh["c_f32"] = cf
    sh["c_onesf"] = onesf
    sh["c_iota"] = np.ascontiguousarray(np.tile(np.arange(NSLOT, dtype=np.float32)[None, :], (128, 1)))
    return sh


def kernel(x, mem, positions, **w):
    x = np.asarray(x, dtype=np.float32)
    mem = np.asarray(mem, dtype=np.float32)
    positions = np.asarray(positions, dtype=np.int32)
    B = x.shape[0]
    nb = B // NCORES
    sh = _prep_shared(**w)
    nc = build_program(nb)
    in_maps = []
    for c in range(NCORES):
        m = dict(sh)
        m["x"] = np.ascontiguousarray(x[c * nb:(c + 1) * nb])
        m["mem"] = np.ascontiguousarray(mem[c * nb:(c + 1) * nb])
        m["pos"] = np.ascontiguousarray(positions[c * nb:(c + 1) * nb])
        in_maps.append(m)
    res = run_bass_kernel_spmd(nc, in_maps, core_ids=list(range(NCORES)))
    return np.concatenate([np.asarray(r["out"], dtype=np.float32) for r in res.results], axis=0)
```
